# Optimizing a Trainium2 kernel written in Bass

```python
import jax, jax.numpy as jnp
from jax import lax
import numpy as np

D_MODEL = 2048
BATCH = 2
SEQ = 16384
DEPTH = 2

HEAD_DIM = 128
GDN_HEADS = 8
HGRN_HEADS = 8
GDN_WIDTH = GDN_HEADS * HEAD_DIM
HGRN_WIDTH = HGRN_HEADS * HEAD_DIM
MIX_WIDTH = GDN_WIDTH + HGRN_WIDTH
N_DIR = 2
CONV_K = 7
CHUNK = 64
N_EXPERTS = 16
EXPERT_FF = 2048
CAPACITY_FACTOR = 2
NORM_EPS = 1e-6
IN_SPLITS = (GDN_WIDTH, GDN_WIDTH, GDN_WIDTH, GDN_WIDTH, N_DIR * GDN_HEADS, N_DIR * GDN_HEADS,
             HGRN_WIDTH, N_DIR * HGRN_WIDTH, HGRN_WIDTH, HGRN_WIDTH)
N_IN = sum(IN_SPLITS)

kernel_name = "hybrid_gdn_hgrn2_ecmoe_encoder"


def rms_norm(x, w):
    xf = x.astype(jnp.float32)
    y = xf * lax.rsqrt(jnp.mean(xf * xf, axis=-1, keepdims=True) + NORM_EPS)
    return (y * w.astype(jnp.float32)).astype(x.dtype)


def gated_rms_norm(o, z, w):
    of = o.astype(jnp.float32)
    y = of * lax.rsqrt(jnp.mean(of * of, axis=-1, keepdims=True) + NORM_EPS)
    return y * w.astype(jnp.float32) * jax.nn.silu(z.astype(jnp.float32))


def l2_normalize(x):
    return x * lax.rsqrt(jnp.sum(x * x, axis=-1, keepdims=True) + NORM_EPS)


def centred_depthwise_conv(x, w):
    c = x.shape[-1]
    return lax.conv_general_dilated(
        x, w[:, None, :].astype(x.dtype), window_strides=(1,),
        padding=[((CONV_K - 1) // 2, CONV_K // 2)],
        dimension_numbers=("NWC", "WIO", "NWC"), feature_group_count=c)


def flip_seq(a):
    return jnp.flip(a, axis=2)


def masked_decay(diff, mask):
    return jnp.where(mask, jnp.exp(jnp.minimum(diff, 0.0)), 0.0)


def gated_delta_chunk_scan(q, k, v, g, beta):
    b, h, t, dk = q.shape
    dv = v.shape[-1]
    n = t // CHUNK
    rs = lambda a: a.reshape(b, h, n, CHUNK, *a.shape[3:])
    q, k, v, g, beta = rs(q), rs(k), rs(v), rs(g), rs(beta)
    gc = jnp.cumsum(g, axis=-1)
    incl = jnp.tril(jnp.ones((CHUNK, CHUNK), dtype=bool))
    strict = jnp.tril(jnp.ones((CHUNK, CHUNK), dtype=bool), k=-1)
    decay = masked_decay(gc[..., :, None] - gc[..., None, :], incl)
    kb = k * beta[..., None]
    a_mat = jnp.where(strict, jnp.einsum("bhntk,bhnsk->bhnts", kb, k) * decay, 0.0)
    lmat = a_mat + jnp.eye(CHUNK, dtype=q.dtype)
    u = lax.linalg.triangular_solve(lmat, v * beta[..., None], left_side=True, lower=True)
    w = lax.linalg.triangular_solve(lmat, kb * jnp.exp(gc)[..., None], left_side=True, lower=True)
    qk = jnp.einsum("bhntk,bhnsk->bhnts", q, k) * decay
    qg = q * jnp.exp(gc)[..., None]
    kg = k * jnp.exp(gc[..., -1:] - gc)[..., None]
    glast = jnp.exp(gc[..., -1])

    def step(state, xs):
        u_n, w_n, qk_n, qg_n, kg_n, gl_n = xs
        v_new = u_n - jnp.einsum("bhtk,bhkv->bhtv", w_n, state)
        o = jnp.einsum("bhtk,bhkv->bhtv", qg_n, state) + jnp.einsum("bhts,bhsv->bhtv", qk_n, v_new)
        state = gl_n[..., None, None] * state + jnp.einsum("bhsk,bhsv->bhkv", kg_n, v_new)
        return state, o

    xs = tuple(jnp.moveaxis(a, 2, 0) for a in (u, w, qk, qg, kg, glast))
    s0 = jnp.zeros((b, h, dk, dv), q.dtype)
    _, o = lax.scan(step, s0, xs)
    return jnp.moveaxis(o, 0, 2).reshape(b, h, t, dv)


def hgrn2_chunk_scan(q, k, v, logf):
    b, h, t, dk = q.shape
    dv = v.shape[-1]
    n = t // CHUNK
    rs = lambda a: jnp.moveaxis(a.reshape(b, h, n, CHUNK, a.shape[-1]), 2, 0)
    incl = jnp.tril(jnp.ones((CHUNK, CHUNK), dtype=bool))[:, :, None]

    def step(state, xs):
        q_n, k_n, v_n, lf_n = xs
        bcum = jnp.cumsum(lf_n, axis=-2)
        decay = masked_decay(bcum[:, :, :, None, :] - bcum[:, :, None, :, :], incl)
        attn = jnp.einsum("bhtsk,bhsk->bhts", decay * q_n[:, :, :, None, :], k_n)
        o = jnp.einsum("bhtk,bhkv->bhtv", q_n * jnp.exp(bcum), state) + jnp.einsum("bhts,bhsv->bhtv", attn, v_n)
        blast = bcum[:, :, -1:, :]
        state = jnp.exp(blast[:, :, 0, :])[..., None] * state + jnp.einsum(
            "bhsk,bhsv->bhkv", k_n * jnp.exp(jnp.minimum(blast - bcum, 0.0)), v_n)
        return state, o

    s0 = jnp.zeros((b, h, dk, dv), q.dtype)
    _, o = lax.scan(step, s0, (rs(q), rs(k), rs(v), rs(logf)))
    return jnp.moveaxis(o, 0, 2).reshape(b, h, t, dv)


def token_mixer(u, w_in, conv_w, a_log, dt_bias, gdn_norm_w, lower_bound, hgrn_norm_w, w_out):
    b, s, _ = u.shape
    f32 = jnp.float32
    proj = jnp.einsum("bsd,dn->bsn", u, w_in)
    aq, ak, av, az, abeta, aalpha, bq, bfg, bi, bg = jnp.split(proj, np.cumsum(IN_SPLITS)[:-1], axis=-1)
    to_heads = lambda a, nh: a.reshape(b, s, nh, HEAD_DIM).astype(f32).transpose(0, 2, 1, 3)

    qkv = jax.nn.silu(centred_depthwise_conv(jnp.concatenate([aq, ak, av], axis=-1), conv_w))
    cq, ck, cv = jnp.split(qkv, 3, axis=-1)
    q_a = l2_normalize(to_heads(cq, GDN_HEADS)) * (HEAD_DIM ** -0.5)
    k_a = l2_normalize(to_heads(ck, GDN_HEADS))
    v_a = to_heads(cv, GDN_HEADS)
    beta = jax.nn.sigmoid(abeta.reshape(b, s, N_DIR, GDN_HEADS).astype(f32)).transpose(2, 0, 3, 1)
    g = (-jnp.exp(a_log.astype(f32)) * jax.nn.softplus(
        aalpha.reshape(b, s, N_DIR, GDN_HEADS).astype(f32) + dt_bias.astype(f32))).transpose(2, 0, 3, 1)
    o_a = gated_delta_chunk_scan(q_a, k_a, v_a, g[0], beta[0]) + flip_seq(gated_delta_chunk_scan(
        flip_seq(q_a), flip_seq(k_a), flip_seq(v_a), flip_seq(g[1]), flip_seq(beta[1])))
    o_a = gated_rms_norm(o_a.transpose(0, 2, 1, 3), az.reshape(b, s, GDN_HEADS, HEAD_DIM), gdn_norm_w)

    lb = lower_bound.astype(f32)
    fpre = bfg.reshape(b, s, N_DIR, HGRN_WIDTH).astype(f32)
    f_gate = lb + (1.0 - lb) * jax.nn.sigmoid(fpre)
    logf = jnp.log(jnp.maximum(f_gate, jnp.finfo(f32).tiny))
    kin = (1.0 - lb) * jax.nn.sigmoid(-fpre)
    dir_heads = lambda a: a.reshape(b, s, N_DIR, HGRN_HEADS, HEAD_DIM).transpose(2, 0, 3, 1, 4)
    logf, kin = dir_heads(logf), dir_heads(kin)
    q_b = to_heads(bq, HGRN_HEADS)
    v_b = to_heads(bi, HGRN_HEADS)
    o_b = hgrn2_chunk_scan(q_b, kin[0], v_b, logf[0]) + flip_seq(hgrn2_chunk_scan(
        flip_seq(q_b), flip_seq(kin[1]), flip_seq(v_b), flip_seq(logf[1])))
    o_b = gated_rms_norm(o_b.transpose(0, 2, 1, 3), bg.reshape(b, s, HGRN_HEADS, HEAD_DIM), hgrn_norm_w)

    mixed = jnp.concatenate([o_a.reshape(b, s, GDN_WIDTH), o_b.reshape(b, s, HGRN_WIDTH)], axis=-1)
    return jnp.einsum("bsm,md->bsd", mixed.astype(u.dtype), w_out)


def expert_choice_moe(u, w_router, w_gate, w_up, w_down):
    b, s, d = u.shape
    cap = CAPACITY_FACTOR * s // N_EXPERTS
    probs = jax.nn.softmax(jnp.einsum("bsd,de->bse", u, w_router).astype(jnp.float32), axis=-1)
    gates, idx = lax.top_k(jnp.swapaxes(probs, 1, 2), cap)
    xs = jax.vmap(lambda ub, ib: ub[ib])(u, idx)
    hid = jax.nn.silu(jnp.einsum("becd,edf->becf", xs, w_gate)) * jnp.einsum("becd,edf->becf", xs, w_up)
    y = jnp.einsum("becf,efd->becd", hid, w_down) * gates[..., None].astype(u.dtype)
    return jax.vmap(lambda yb, ib: jnp.zeros((s, d), yb.dtype).at[ib.reshape(-1)].add(yb.reshape(-1, d)))(y, idx)


def setup_inputs(seed: int = 0) -> dict:
    key = jax.random.key(seed)
    ks = jax.random.split(key, 20)
    f32 = jnp.float32
    nrm = lambda k, shape, scale: jax.random.normal(k, shape, f32) * scale
    dt = jnp.exp(jax.random.uniform(ks[6], (DEPTH, N_DIR, GDN_HEADS), f32) * (np.log(0.1) - np.log(0.001)) + np.log(0.001))
    return {
        "x": jax.random.normal(ks[0], (BATCH, SEQ, D_MODEL), f32),
        "norm_mix": 1.0 + nrm(ks[1], (DEPTH, D_MODEL), 0.02),
        "norm_ffn": 1.0 + nrm(ks[2], (DEPTH, D_MODEL), 0.02),
        "norm_final": 1.0 + nrm(ks[3], (D_MODEL,), 0.02),
        "w_in": nrm(ks[4], (DEPTH, D_MODEL, N_IN), D_MODEL ** -0.5),
        "conv_w": nrm(ks[5], (DEPTH, CONV_K, 3 * GDN_WIDTH), CONV_K ** -0.5),
        "gdn_a_log": jnp.log(jax.random.uniform(ks[7], (DEPTH, N_DIR, GDN_HEADS), f32, 1.0, 16.0)),
        "gdn_dt_bias": dt + jnp.log(-jnp.expm1(-dt)),
        "gdn_norm": 1.0 + nrm(ks[8], (DEPTH, HEAD_DIM), 0.02),
        "hgrn_lower_bounds": nrm(ks[9], (DEPTH, N_DIR, HGRN_WIDTH), 0.5),
        "hgrn_norm": 1.0 + nrm(ks[10], (DEPTH, HEAD_DIM), 0.02),
        "w_out": nrm(ks[11], (DEPTH, MIX_WIDTH, D_MODEL), MIX_WIDTH ** -0.5),
        "w_router": nrm(ks[12], (DEPTH, D_MODEL, N_EXPERTS), D_MODEL ** -0.5),
        "w_gate": nrm(ks[13], (DEPTH, N_EXPERTS, D_MODEL, EXPERT_FF), D_MODEL ** -0.5),
        "w_up": nrm(ks[14], (DEPTH, N_EXPERTS, D_MODEL, EXPERT_FF), D_MODEL ** -0.5),
        "w_down": nrm(ks[15], (DEPTH, N_EXPERTS, EXPERT_FF, D_MODEL), EXPERT_FF ** -0.5),
    }


def reference(x, norm_mix, norm_ffn, norm_final, w_in, conv_w, gdn_a_log, gdn_dt_bias, gdn_norm,
              hgrn_lower_bounds, hgrn_norm, w_out, w_router, w_gate, w_up, w_down):
    p = jax.nn.softmax(hgrn_lower_bounds.astype(jnp.float32), axis=0)
    lower_bound = jnp.cumsum(p, axis=0) - p[0]
    h = x
    for l in range(DEPTH):
        u = rms_norm(h, norm_mix[l])
        h = h + token_mixer(u, w_in[l], conv_w[l], gdn_a_log[l], gdn_dt_bias[l], gdn_norm[l],
                            lower_bound[l], hgrn_norm[l], w_out[l])
        u = rms_norm(h, norm_ffn[l])
        h = h + expert_choice_moe(u, w_router[l], w_gate[l], w_up[l], w_down[l])
    return rms_norm(h, norm_final)
```

```python
import contextlib
import numpy as np
import concourse.bass as bass
import concourse.mybir as mybir
from concourse.bass_utils import run_bass_kernel_spmd

F32 = mybir.dt.float32
BF16 = mybir.dt.bfloat16
I32 = mybir.dt.int32
AF = mybir.ActivationFunctionType
ALU = mybir.AluOpType
AX = mybir.AxisListType


class Buf:
    __slots__ = ("name", "last_w", "readers", "excl")

    def __init__(self, name="", excl=False):
        self.name = name
        self.last_w = None
        self.readers = {}
        self.excl = excl


class Eng:
    def __init__(self, name, handle, sem, is_pe=False):
        self.name = name
        self.h = handle
        self.sem = sem
        self.count = 0
        self.waited = {}
        self.is_pe = is_pe
        self.dma_sems = []
        self.dma_tot = []
        self.dma_rr = 0


class Sched:
    def __init__(self, nc, stack, n_dma_sems=6):
        self.nc = nc
        self.stack = stack
        mk = lambda n: stack.enter_context(nc.semaphore(n))
        self.pe = Eng("pe", nc.tensor, mk("s_pe"), is_pe=True)
        self.act = Eng("act", nc.scalar, mk("s_act"))
        self.dve = Eng("dve", nc.vector, mk("s_dve"))
        self.pool = Eng("pool", nc.gpsimd, mk("s_pool"))
        self.sp = Eng("sp", nc.sync, mk("s_sp"))
        for e in (self.sp, self.pool, self.act):
            for j in range(n_dma_sems):
                e.dma_sems.append(mk(f"d_{e.name}{j}"))
                e.dma_tot.append(0)
        self.semkey = {}
        self.n_ops = 0
        self.prog = {e.name: [] for e in (self.pe, self.act, self.dve, self.pool, self.sp)}
        self.cur_waits = None

    def sb(self, name, shape, dt=F32, stack=None):
        t = (stack or self.stack).enter_context(self.nc.sbuf_tensor(name, list(shape), dt))
        return t

    def barrier(self):
        engs = (self.pe, self.act, self.dve, self.pool, self.sp)
        toks = []
        for e in engs:
            if e.count:
                toks.append((e.sem, e.count))
            for sem, tot in zip(e.dma_sems, e.dma_tot):
                if tot:
                    toks.append((sem, tot))
        for e in engs:
            for sem, val in toks:
                self._wait(e, sem, val)

    def ps(self, name, shape, dt=F32):
        t = self.stack.enter_context(self.nc.psum_tensor(name, list(shape), dt))
        return t

    def _wait(self, eng, sem, val):
        k = id(sem)
        if eng.waited.get(k, 0) >= val:
            return
        self.prog[eng.name].append(("wait", sem, val))
        eng.waited[k] = val

    def op(self, eng, fn, reads=(), writes=(), dma=False):
        ex = [b for b in reads if b.excl]
        if ex:
            reads = [b for b in reads if not b.excl]
            writes = list(writes) + [b for b in ex if b not in writes]
        deps = {}

        def add(tok):
            if tok is None:
                return
            sem, val = tok
            k = id(sem)
            if k not in deps or deps[k][1] < val:
                deps[k] = (sem, val)

        for b in reads:
            add(b.last_w)
        for b in writes:
            add(b.last_w)
            for tok in b.readers.values():
                add(tok)
        for k, (sem, val) in deps.items():
            if eng.is_pe and sem is eng.sem:
                continue
            self._wait(eng, sem, val)
        if dma:
            j = eng.dma_rr
            eng.dma_rr = (j + 1) % len(eng.dma_sems)
            sem = eng.dma_sems[j]
            if eng.dma_tot[j] > 0:
                self._wait(eng, sem, eng.dma_tot[j])
            self.prog[eng.name].append(("op", fn, sem, 16))
            eng.dma_tot[j] += 16
            tok = (sem, eng.dma_tot[j])
        else:
            self.prog[eng.name].append(("op", fn, eng.sem, 1))
            eng.count += 1
            tok = (eng.sem, eng.count)
        for b in reads:
            b.readers[id(tok[0])] = tok
        for b in writes:
            b.last_w = tok
            b.readers = {}
        self.n_ops += 1
        return tok

    def dma(self, eng, out, in_, reads=(), writes=(), **kw):
        return self.op(eng, ("dma_start", dict(out=out, in_=in_, **kw)), reads, writes, dma=True)

    def emit(self):
        nc = self.nc
        def run(h, items):
            for it in items:
                if it[0] == "wait":
                    h.wait_ge(it[1], it[2])
                else:
                    name, kw = it[1]
                    ins = getattr(h, name)(**kw)
                    ins.then_inc(it[2], it[3])
        with nc.Block() as block:
            @block.tensor
            def _(h):
                run(h, self.prog["pe"])
            @block.scalar
            def _(h):
                run(h, self.prog["act"])
            @block.vector
            def _(h):
                run(h, self.prog["dve"])
            @block.gpsimd
            def _(h):
                run(h, self.prog["pool"])
            @block.sync
            def _(h):
                run(h, self.prog["sp"])

    def finish(self, bufs):
        for b in bufs:
            if b.last_w is not None:
                self._wait(self.sp, b.last_w[0], b.last_w[1])


D = 2048
KC = D // 128
HD = 128
C = 64


def build_mixer(T, n_g, n_h, debug_outputs=(), layer=0, depth=2):
    nc = bass.Bass("TRN2", target_bir_lowering=False)
    NB = T // 512
    fm_tiles = []
    for h in range(n_g):
        fm_tiles += [(f"gq{h}", 128), (f"gk{h}", 128), (f"gv{h}", 128), (f"gb{h}", 2), (f"ga{h}", 2)]
    for h in range(n_h):
        fm_tiles += [(f"hq{h}", 128), (f"hf0{h}", 128), (f"hf1{h}", 128)]
    NFM = sum(n for _, n in fm_tiles)
    NTM = n_g * 128 + n_h * 256
    uT = nc.dram_tensor("uT", [D, T], BF16, kind="ExternalInput").ap()
    wfm = nc.dram_tensor("wfm", [D, NFM], F32, kind="ExternalInput").ap()
    wtm = nc.dram_tensor("wtm", [D, NTM], F32, kind="ExternalInput").ap()
    NCST = 768
    consts = nc.dram_tensor("consts", [128, NCST], F32, kind="ExternalInput").ap()
    if n_g:
        gcw = nc.dram_tensor("gcw", [128, n_g * 21], F32, kind="ExternalInput").ap()
        gpar = nc.dram_tensor("gpar", [2, n_g * 2], F32, kind="ExternalInput").ap()
    normw = nc.dram_tensor("normw", [128, 256], F32, kind="ExternalInput").ap()
    DEPTH_ = depth; LAYER_ = layer
    if n_h:
        hlb = nc.dram_tensor("hlb", [128, n_h * 2 * depth], F32, kind="ExternalInput").ap()
        hcm = nc.dram_tensor("hcm", [128, depth], F32, kind="ExternalInput").ap()
    dbg = {}
    ALLOUT = []
    def scratch(name, shape, dt=F32):
        kind = "ExternalOutput" if name in debug_outputs else "Internal"
        t = nc.dram_tensor(name, list(shape), dt, kind=kind).ap()
        dbg[name] = t
        return t
    FMs = {}
    for name, n in fm_tiles:
        FMs[name] = scratch("fm_" + name, [n, T])
    TMs = scratch("tm", [T, NTM])
    B_FM = {name: Buf() for name, _ in fm_tiles}
    B_TM = Buf()

    with contextlib.ExitStack() as st:
        S = Sched(nc, st)
        PS = [S.ps(f"ps{i}", [128, 512]) for i in range(8)]
        BPS = [Buf(f"ps{i}", excl=True) for i in range(8)]
        phA = contextlib.ExitStack()
        wfm_sb = S.sb("wfm_sb", [128, KC, NFM], BF16, phA); Bwfm = Buf()
        wtm_sb = S.sb("wtm_sb", [128, KC, NTM], BF16, phA); Bwtm = Buf()
        for kc in range(KC):
            S.dma(S.pool, wfm_sb[:, kc, :], wfm[kc * 128:(kc + 1) * 128, :], writes=[Bwfm])
            S.dma(S.pool, wtm_sb[:, kc, :], wtm[kc * 128:(kc + 1) * 128, :], writes=[Bwtm])
        ut = [S.sb(f"ut{i}", [128, KC, 512], BF16, phA) for i in range(2)]; But = [Buf() for _ in range(2)]
        stg = [S.sb(f"stg{i}", [128, 512], F32, phA) for i in range(4)]; Bstg = [Buf() for _ in range(4)]
        uTv = uT.rearrange("(kc p) t -> p kc t", p=128)
        nstg = 0
        npb = 0
        for blk in range(NB):
            ub = blk % 2
            S.dma(S.sp, ut[ub][:], uTv[:, :, blk * 512:(blk + 1) * 512], writes=[But[ub]])
            col = 0
            for name, n in fm_tiles:
                pb = npb % 4; npb += 1
                for kc in range(KC):
                    S.op(S.pe, ("matmul", dict(out=PS[pb][0:n, :], lhsT=wfm_sb[:, kc, col:col + n], rhs=ut[ub][:, kc, :],
                                               start=(kc == 0), stop=(kc == KC - 1))),
                         reads=[Bwfm, But[ub]], writes=[BPS[pb]])
                sg = nstg % 4; nstg += 1
                eng = S.act if sg % 2 == 0 else S.dve
                if eng is S.act:
                    S.op(S.act, ("copy", dict(out=stg[sg][0:n, :], in_=PS[pb][0:n, :])), reads=[BPS[pb]], writes=[Bstg[sg]])
                else:
                    S.op(S.dve, ("tensor_copy", dict(out=stg[sg][0:n, :], in_=PS[pb][0:n, :])), reads=[BPS[pb]], writes=[Bstg[sg]])
                S.dma(S.sp, FMs[name][:, blk * 512:(blk + 1) * 512], stg[sg][0:n, :], reads=[Bstg[sg]], writes=[B_FM[name]])
                col += n
            for tt in range(4):
                t0 = blk * 512 + tt * 128
                for c0 in range(0, NTM, 512):
                    cn = min(512, NTM - c0)
                    pb = npb % 4; npb += 1
                    for kc in range(KC):
                        S.op(S.pe, ("matmul", dict(out=PS[pb][:, 0:cn], lhsT=ut[ub][:, kc, tt * 128:(tt + 1) * 128], rhs=wtm_sb[:, kc, c0:c0 + cn],
                                                   start=(kc == 0), stop=(kc == KC - 1))),
                             reads=[Bwtm, But[ub]], writes=[BPS[pb]])
                    sg = nstg % 4; nstg += 1
                    if sg % 2 == 0:
                        S.op(S.act, ("copy", dict(out=stg[sg][:, 0:cn], in_=PS[pb][:, 0:cn])), reads=[BPS[pb]], writes=[Bstg[sg]])
                    else:
                        S.op(S.dve, ("tensor_copy", dict(out=stg[sg][:, 0:cn], in_=PS[pb][:, 0:cn])), reads=[BPS[pb]], writes=[Bstg[sg]])
                    S.dma(S.sp, TMs[t0:t0 + 128, c0:c0 + cn], stg[sg][:, 0:cn], reads=[Bstg[sg]], writes=[B_TM])
        S.barrier()
        phA.close()
        cst = S.sb("cst", [128, NCST]); Bc = Buf()
        S.dma(S.sp, cst[:], consts[:, :], writes=[Bc])
        ident = cst[:, 0:128]
        ones = cst[:, 128:256]
        def TRI(d): return cst[0:64, 256 + d * 64: 256 + d * 64 + 64]
        def NTRI(d): return cst[0:64, 384 + d * 64: 384 + d * 64 + 64]
        def NEGM(d): return cst[0:64, 512 + d * 64: 512 + d * 64 + 64]
        def SMASK(d): return cst[0:64, 640 + d * 64: 640 + d * 64 + 64]
        I64 = cst[0:64, 0:64]
        def bc8(ap64):
            return ap64.unsqueeze(1).broadcast_to([64, 8, 64])
        def v8(t):
            return t.rearrange("p (a b) -> p a b", b=64)
        NCH = T // C
        NG = NCH // 8
        pbc = [0]
        def nextpb(lo=0, hi=8):
            pbc[0] += 1
            return lo + pbc[0] % (hi - lo)

        gsm = {}
        for h in range(n_g):
            for d in range(2):
                gsm[(h, d)] = dict(
                    b=S.sb(f"g_b{h}{d}", [64, NCH]), neg=S.sb(f"g_neg{h}{d}", [64, NCH]),
                    bkd=S.sb(f"g_bkd{h}{d}", [64, NCH]), gl=S.sb(f"g_gl{h}{d}", [128, NCH]), B=Buf())
        hsm = {}
        for h in range(n_h):
            for d in range(2):
                hsm[(h, d)] = dict(el=S.sb(f"h_el{h}{d}", [128, NCH]), B=Buf())

        TB = min(T, 2048)
        NTB = T // TB
        KhT = {}; QhT = {}; Ktok = {}; Vtok = {}; GB = {}
        BKhT = {}; BQhT = {}; BKtok = {}; BVtok = {}; BGB = {}
        for h in range(n_g):
            KhT[h] = scratch(f"KhT{h}", [128, T], BF16); QhT[h] = scratch(f"QhT{h}", [128, T], BF16)
            Ktok[h] = scratch(f"Ktok{h}", [T, 128], BF16); Vtok[h] = scratch(f"Vtok{h}", [T, 128], F32)
            GB[h] = scratch(f"GB{h}", [4, T], F32)
            BKhT[h] = Buf(); BQhT[h] = Buf(); BKtok[h] = Buf(); BVtok[h] = Buf(); BGB[h] = Buf()
            ALLOUT += [BKhT[h], BQhT[h], BKtok[h], BVtok[h], BGB[h]]
        if n_g:
          with contextlib.ExitStack() as ph:
            cw = S.sb("cw", [128, n_g * 21], F32, ph); Bcw = Buf()
            S.dma(S.sp, cw[:], gcw[:, :], writes=[Bcw])
            gp = S.sb("gp", [2, n_g * 2], F32, ph); Bgp = Buf()
            S.dma(S.sp, gp[:], gpar[:, :], writes=[Bgp])
            xin = [S.sb(f"xin{i}", [128, TB + 6], F32, ph) for i in range(2)]; Bxin = [Buf(), Buf()]
            acc = S.sb("acc", [128, TB], F32, ph); Bacc = Buf()
            yy = S.sb("yy", [128, TB], F32, ph); Byy = Buf()
            sq = S.sb("sq", [128, TB], F32, ph); Bsq = Buf()
            kh = S.sb("kh", [128, TB], F32, ph); Bkh = Buf()
            obf = [S.sb(f"obf{i}", [128, TB], BF16, ph) for i in range(2)]; Bobf = [Buf(), Buf()]
            rs = S.sb("rs", [128, 512], F32, ph); Brs = Buf()
            tst = [S.sb(f"tst{i}", [128, 512], F32, ph) for i in range(2)]; Btst = [Buf(), Buf()]
            tsb = [S.sb(f"tsb{i}", [128, 512], BF16, ph) for i in range(2)]; Btsb = [Buf(), Buf()]
            BT = min(T, 4096)
            bt = S.sb("bt", [2, BT], F32, ph); Bbt = Buf()
            at = S.sb("at", [2, BT], F32, ph); Bat = Buf()
            na = S.sb("na", [2, 2], F32, ph); Bna = Buf()
            nx = 0; nob = 0; nts = 0
            for h in range(n_g):
                for j, nm in enumerate(("gq", "gk", "gv")):
                    src = FMs[f"{nm}{h}"]
                    for tb in range(NTB):
                        t0 = tb * TB
                        xi = nx % 2; nx += 1
                        lo = max(t0 - 3, 0); hi = min(t0 + TB + 3, T)
                        if t0 == 0:
                            S.op(S.pool, ("memset", dict(ap=xin[xi][:, 0:3], constant=0.0)), writes=[Bxin[xi]])
                        if t0 + TB == T:
                            S.op(S.pool, ("memset", dict(ap=xin[xi][:, TB + 3:TB + 6], constant=0.0)), writes=[Bxin[xi]])
                        S.dma(S.sp, xin[xi][:, lo - (t0 - 3): hi - (t0 - 3)], src[:, lo:hi], reads=[B_FM[f"{nm}{h}"]], writes=[Bxin[xi]])
                        cb = h * 21 + j * 7
                        S.op(S.dve, ("tensor_scalar", dict(out=acc[:], in0=xin[xi][:, 0:TB], scalar1=cw[:, cb:cb + 1], scalar2=None, op0=ALU.mult)),
                             reads=[Bxin[xi], Bcw], writes=[Bacc])
                        for tap in range(1, 7):
                            S.op(S.dve, ("scalar_tensor_tensor", dict(out=acc[:], in0=xin[xi][:, tap:tap + TB], scalar=cw[:, cb + tap:cb + tap + 1], in1=acc[:], op0=ALU.mult, op1=ALU.add)),
                                 reads=[Bxin[xi], Bcw, Bacc], writes=[Bacc])
                        S.op(S.act, ("activation", dict(out=yy[:], in_=acc[:], func=AF.Silu)), reads=[Bacc], writes=[Byy])
                        if nm == "gv":
                            tsrc, Btsrc = yy, Byy
                        else:
                            S.op(S.act, ("activation", dict(out=sq[:], in_=yy[:], func=AF.Square)), reads=[Byy], writes=[Bsq])
                            ob = nob % 2; nob += 1
                            for sbk in range(TB // 512):
                                sl = slice(sbk * 512, (sbk + 1) * 512)
                                pb = nextpb()
                                S.op(S.pe, ("matmul", dict(out=PS[pb][:, :], lhsT=ones, rhs=sq[:, sl], start=True, stop=True)), reads=[Bc, Bsq], writes=[BPS[pb]])
                                S.op(S.dve, ("tensor_scalar", dict(out=rs[:], in0=PS[pb][:, :], scalar1=1e-6, scalar2=None, op0=ALU.add)), reads=[BPS[pb]], writes=[Brs])
                                S.op(S.act, ("sqrt", dict(out=rs[:], in_=rs[:])), reads=[Brs], writes=[Brs])
                                S.op(S.dve, ("reciprocal", dict(out=rs[:], in_=rs[:])), reads=[Brs], writes=[Brs])
                                if nm == "gk":
                                    S.op(S.dve, ("tensor_tensor", dict(out=kh[:, sl], in0=yy[:, sl], in1=rs[:], op=ALU.mult)), reads=[Byy, Brs], writes=[Bkh])
                                else:
                                    S.op(S.dve, ("scalar_tensor_tensor", dict(out=obf[ob][:, sl], in0=yy[:, sl], scalar=float(HD) ** -0.5, in1=rs[:], op0=ALU.mult, op1=ALU.mult)),
                                         reads=[Byy, Brs], writes=[Bobf[ob]])
                            if nm == "gk":
                                S.op(S.act, ("copy", dict(out=obf[ob][:], in_=kh[:])), reads=[Bkh], writes=[Bobf[ob]])
                                S.dma(S.sp, KhT[h][:, t0:t0 + TB], obf[ob][:], reads=[Bobf[ob]], writes=[BKhT[h]])
                                tsrc, Btsrc = kh, Bkh
                            else:
                                S.dma(S.sp, QhT[h][:, t0:t0 + TB], obf[ob][:], reads=[Bobf[ob]], writes=[BQhT[h]])
                                tsrc = None
                        if tsrc is not None:
                            for sbk in range(TB // 512):
                                pb = nextpb()
                                for jj in range(4):
                                    c0 = sbk * 512 + jj * 128
                                    S.op(S.pe, ("transpose", dict(out=PS[pb][:, jj * 128:(jj + 1) * 128], in_=tsrc[:, c0:c0 + 128], identity=ident)),
                                         reads=[Bc, Btsrc], writes=[BPS[pb]])
                                ts = nts % 2; nts += 1
                                tt0 = t0 + sbk * 512
                                if nm == "gv":
                                    S.op(S.act, ("copy", dict(out=tst[ts][:], in_=PS[pb][:, :])), reads=[BPS[pb]], writes=[Btst[ts]])
                                    S.dma(S.sp, Vtok[h][tt0:tt0 + 512, :].rearrange("(j p) d -> p j d", p=128), tst[ts][:].rearrange("p (j d) -> p j d", d=128),
                                          reads=[Btst[ts]], writes=[BVtok[h]])
                                else:
                                    S.op(S.act, ("copy", dict(out=tsb[ts][:], in_=PS[pb][:, :])), reads=[BPS[pb]], writes=[Btsb[ts]])
                                    S.dma(S.sp, Ktok[h][tt0:tt0 + 512, :].rearrange("(j p) d -> p j d", p=128), tsb[ts][:].rearrange("p (j d) -> p j d", d=128),
                                          reads=[Btsb[ts]], writes=[BKtok[h]])
                S.op(S.act, ("activation", dict(out=na[:, 0:1], in_=gp[:, h * 2 + 1:h * 2 + 2], func=AF.Exp)), reads=[Bgp], writes=[Bna])
                S.op(S.dve, ("tensor_scalar", dict(out=na[:, 1:2], in0=na[:, 0:1], scalar1=-1.0, scalar2=None, op0=ALU.mult)), reads=[Bna], writes=[Bna])
                for t0 in range(0, T, BT):
                    S.dma(S.sp, bt[:], FMs[f"gb{h}"][:, t0:t0 + BT], reads=[B_FM[f"gb{h}"]], writes=[Bbt])
                    S.op(S.act, ("activation", dict(out=bt[:], in_=bt[:], func=AF.Sigmoid)), reads=[Bbt], writes=[Bbt])
                    S.dma(S.sp, GB[h][0:2, t0:t0 + BT], bt[:], reads=[Bbt], writes=[BGB[h]])
                    S.dma(S.sp, at[:], FMs[f"ga{h}"][:, t0:t0 + BT], reads=[B_FM[f"ga{h}"]], writes=[Bat])
                    S.op(S.act, ("activation", dict(out=at[:], in_=at[:], func=AF.Exp, bias=gp[:, h * 2:h * 2 + 1])), reads=[Bat, Bgp], writes=[Bat])
                    S.op(S.act, ("activation", dict(out=at[:], in_=at[:], func=AF.Ln, bias=1.0)), reads=[Bat], writes=[Bat])
                    S.op(S.dve, ("tensor_scalar", dict(out=at[:], in0=at[:], scalar1=na[:, 1:2], scalar2=None, op0=ALU.mult)), reads=[Bat, Bna], writes=[Bat])
                    S.dma(S.sp, GB[h][2:4, t0:t0 + BT], at[:], reads=[Bat], writes=[BGB[h]])
          S.barrier()
        QG = {}; ZT = {}; QKM = {}; OG = {}
        BQG = {}; BZT = {}; BQKM = {}; BOG = {}
        for h in range(n_g):
            for d in range(2):
                QG[(h, d)] = scratch(f"QG{h}{d}", [128, T], BF16)
                ZT[(h, d)] = scratch(f"ZT{h}{d}", [NG, 64, 512], BF16)
                QKM[(h, d)] = scratch(f"QKM{h}{d}", [NG, 64, 512], BF16)
                OG[(h, d)] = scratch(f"OG{h}{d}", [T, 128], F32)
                BQG[(h, d)] = Buf(); BZT[(h, d)] = Buf(); BQKM[(h, d)] = Buf(); BOG[(h, d)] = Buf()
                ALLOUT += [BQG[(h, d)], BZT[(h, d)], BQKM[(h, d)]]
        if n_g:
          with contextlib.ExitStack() as ph:
            g_sn = S.sb("g_sn", [64, NCH], F32, ph); Bg = Buf()
            gc_sb = S.sb("gc_sb", [64, NCH], F32, ph); Bgc = Buf()
            tmpT = [S.sb(f"tmpT{i}", [128, 64], F32, ph) for i in range(2)]; BtmpT = [Buf(), Buf()]
            kg_ = [S.sb(f"kg{i}", [128, 512], BF16, ph) for i in range(2)]; Bkg = [Buf(), Buf()]
            qg_ = [S.sb(f"qgi{i}", [128, 512], BF16, ph) for i in range(2)]; Bqg = [Buf(), Buf()]
            gb8 = S.sb("gb8", [64, 8 * 128], F32, ph); Bgb8 = Buf()
            egcb = S.sb("egcb", [128, 512], F32, ph); Begcb = Buf()
            qgo = [S.sb(f"qgo{i}", [128, 512], BF16, ph) for i in range(2)]; Bqgo = [Buf(), Buf()]
            Dm = S.sb("Dm", [64, 512], F32, ph); BDm = Buf()
            t1 = S.sb("t1", [64, 512], F32, ph); Bt1 = Buf()
            qkm = [S.sb(f"qkm{i}", [64, 512], BF16, ph) for i in range(2)]; Bqkm = [Buf(), Buf()]
            A_ = [S.sb(f"A{i}", [64, 512], F32, ph) for i in range(2)]; BA = [Buf(), Buf()]
            AT_ = [S.sb(f"AT{i}", [64, 512], F32, ph) for i in range(2)]; BAT = [Buf(), Buf()]
            Pm = S.sb("Pm", [64, 512], F32, ph); BP = Buf()
            zto = [S.sb(f"zto{i}", [64, 512], BF16, ph) for i in range(2)]; Bzto = [Buf(), Buf()]
            ntm_ = 0; ngrp = 0
            for h in range(n_g):
                for d in range(2):
                    sm = gsm[(h, d)]
                    for r, dst, Bdst in ((d, sm["b"], sm["B"]), (2 + d, g_sn, Bg)):
                        for n0 in range(0, NCH, 128):
                            nn = min(128, NCH - n0)
                            ti = ntm_ % 2; ntm_ += 1
                            S.dma(S.sp, tmpT[ti][0:nn, :], GB[h][r, n0 * 64:(n0 + nn) * 64].rearrange("(n s) -> n s", s=64), reads=[BGB[h]], writes=[BtmpT[ti]])
                            pb = nextpb()
                            S.op(S.pe, ("transpose", dict(out=PS[pb][0:64, 0:nn], in_=tmpT[ti][0:nn, :], identity=cst[0:nn, 0:nn])), reads=[Bc, BtmpT[ti]], writes=[BPS[pb]])
                            S.op(S.act, ("copy", dict(out=dst[:, n0:n0 + nn], in_=PS[pb][0:64, 0:nn])), reads=[BPS[pb]], writes=[Bdst])
                    pa = nextpb(); pbk = nextpb()
                    S.op(S.pe, ("matmul", dict(out=PS[pa][0:64, 0:NCH], lhsT=TRI(d), rhs=g_sn[:], start=True, stop=True)), reads=[Bc, Bg], writes=[BPS[pa]])
                    S.op(S.pe, ("matmul", dict(out=PS[pbk][:, 0:NCH], lhsT=cst[0:64, 128:256], rhs=g_sn[:], start=True, stop=True)), reads=[Bc, Bg], writes=[BPS[pbk]])
                    S.op(S.act, ("copy", dict(out=gc_sb[:], in_=PS[pa][0:64, 0:NCH])), reads=[BPS[pa]], writes=[Bgc])
                    S.op(S.act, ("activation", dict(out=sm["gl"][:], in_=PS[pbk][:, 0:NCH], func=AF.Exp)), reads=[BPS[pbk]], writes=[sm["B"]])
                    S.op(S.act, ("activation", dict(out=sm["neg"][:], in_=gc_sb[:], func=AF.Exp)), reads=[Bgc], writes=[sm["B"]])
                    S.op(S.dve, ("tensor_scalar", dict(out=sm["neg"][:], in0=sm["neg"][:], scalar1=-1.0, scalar2=None, op0=ALU.mult)), reads=[sm["B"]], writes=[sm["B"]])
                    S.op(S.dve, ("tensor_tensor", dict(out=sm["bkd"][:], in0=PS[pbk][0:64, 0:NCH], in1=gc_sb[:], op=ALU.subtract)), reads=[BPS[pbk], Bgc], writes=[sm["B"]])
                    S.op(S.act, ("activation", dict(out=sm["bkd"][:], in_=sm["bkd"][:], func=AF.Exp)), reads=[sm["B"]], writes=[sm["B"]])
                    S.op(S.dve, ("tensor_tensor", dict(out=sm["bkd"][:], in0=sm["bkd"][:], in1=sm["b"][:], op=ALU.mult)), reads=[sm["B"]], writes=[sm["B"]])
                    for cg in range(NG):
                        gi = ngrp % 2; ngrp += 1
                        c0 = cg * 8
                        tk = slice(cg * 512, (cg + 1) * 512)
                        S.dma(S.sp, kg_[gi][:], KhT[h][:, tk], reads=[BKhT[h]], writes=[Bkg[gi]])
                        S.dma(S.sp, qg_[gi][:], QhT[h][:, tk], reads=[BQhT[h]], writes=[Bqg[gi]])
                        gb8v = gb8[:].rearrange("p (a b) -> p a b", b=128)
                        S.op(S.dve, ("tensor_copy", dict(out=gb8v, in_=g_sn[:, c0:c0 + 8].unsqueeze(2).broadcast_to([64, 8, 128]))), reads=[Bg], writes=[Bgb8])
                        p1 = nextpb()
                        for i in range(8):
                            S.op(S.pe, ("matmul", dict(out=PS[p1][:, i * 64:(i + 1) * 64], lhsT=gb8[:, i * 128:(i + 1) * 128], rhs=TRI(d), start=True, stop=True)),
                                 reads=[Bgb8, Bc], writes=[BPS[p1]])
                        S.op(S.act, ("activation", dict(out=egcb[:], in_=PS[p1][:, :], func=AF.Exp)), reads=[BPS[p1]], writes=[Begcb])
                        S.op(S.dve, ("tensor_tensor", dict(out=qgo[gi][:], in0=qg_[gi][:], in1=egcb[:], op=ALU.mult)), reads=[Bqg[gi], Begcb], writes=[Bqgo[gi]])
                        S.dma(S.sp, QG[(h, d)][:, tk], qgo[gi][:], reads=[Bqgo[gi]], writes=[BQG[(h, d)]])
                        p2 = nextpb()
                        for i in range(8):
                            o_ = PS[p2][0:64, i * 64:(i + 1) * 64]
                            gbi = gb8[:, i * 128:i * 128 + 64]
                            S.op(S.pe, ("matmul", dict(out=o_, lhsT=gbi, rhs=TRI(d), start=True, stop=False)), reads=[Bgb8, Bc], writes=[BPS[p2]])
                            S.op(S.pe, ("matmul", dict(out=o_, lhsT=NTRI(d), rhs=gbi, start=False, stop=False)), reads=[Bgb8, Bc], writes=[BPS[p2]])
                            S.op(S.pe, ("matmul", dict(out=o_, lhsT=I64, rhs=NEGM(d), start=False, stop=True)), reads=[Bc], writes=[BPS[p2]])
                        S.op(S.act, ("activation", dict(out=Dm[:], in_=PS[p2][0:64, :], func=AF.Exp)), reads=[BPS[p2]], writes=[BDm])
                        p3 = nextpb(); p4 = nextpb()
                        for i in range(8):
                            ck = slice(i * 64, (i + 1) * 64)
                            S.op(S.pe, ("matmul", dict(out=PS[p3][0:64, ck], lhsT=kg_[gi][:, ck], rhs=kg_[gi][:, ck], start=True, stop=True)), reads=[Bkg[gi]], writes=[BPS[p3]])
                        for i in range(8):
                            ck = slice(i * 64, (i + 1) * 64)
                            S.op(S.pe, ("matmul", dict(out=PS[p4][0:64, ck], lhsT=kg_[gi][:, ck], rhs=qg_[gi][:, ck], start=True, stop=True)), reads=[Bkg[gi], Bqg[gi]], writes=[BPS[p4]])
                        S.op(S.dve, ("tensor_tensor", dict(out=t1[:], in0=PS[p3][0:64, :], in1=Dm[:], op=ALU.mult)), reads=[BPS[p3], BDm], writes=[Bt1])
                        S.op(S.dve, ("tensor_tensor", dict(out=qkm[gi][:], in0=PS[p4][0:64, :], in1=Dm[:], op=ALU.mult)), reads=[BPS[p4], BDm], writes=[Bqkm[gi]])
                        S.dma(S.sp, QKM[(h, d)][cg], qkm[gi][:], reads=[Bqkm[gi]], writes=[BQKM[(h, d)]])
                        S.op(S.pool, ("tensor_tensor", dict(out=v8(t1[:]), in0=v8(t1[:]), in1=sm["b"][:, c0:c0 + 8].unsqueeze(2).broadcast_to([64, 8, 64]), op=ALU.mult)),
                             reads=[Bt1, sm["B"]], writes=[Bt1])
                        A = A_[0]; AT = AT_[0]; BAc = BA[0]; BATc = BAT[0]
                        S.op(S.pool, ("tensor_tensor", dict(out=v8(A[:]), in0=v8(t1[:]), in1=bc8(SMASK(d)), op=ALU.mult)), reads=[Bt1, Bc], writes=[BAc])
                        p5 = nextpb()
                        for i in range(8):
                            ck = slice(i * 64, (i + 1) * 64)
                            S.op(S.pe, ("transpose", dict(out=PS[p5][0:64, ck], in_=A[:, ck], identity=I64)), reads=[BAc, Bc], writes=[BPS[p5]])
                        S.op(S.act, ("copy", dict(out=AT[:], in_=PS[p5][0:64, :])), reads=[BPS[p5]], writes=[BATc])
                        S.op(S.dve, ("scalar_tensor_tensor", dict(out=v8(Pm[:]), in0=v8(A[:]), scalar=-1.0, in1=bc8(I64), op0=ALU.mult, op1=ALU.add)), reads=[BAc, Bc], writes=[BP])
                        cur = 0
                        for lvl in range(5):
                            nxt = 1 - cur
                            A, AT, A2, A2T = A_[cur], AT_[cur], A_[nxt], AT_[nxt]
                            px = nextpb(); py = nextpb(); pz = nextpb()
                            for i in range(8):
                                ck = slice(i * 64, (i + 1) * 64)
                                S.op(S.pe, ("matmul", dict(out=PS[px][0:64, ck], lhsT=A[:, ck], rhs=AT[:, ck], start=True, stop=True)), reads=[BA[cur], BAT[cur]], writes=[BPS[px]])
                            if lvl < 4:
                                for i in range(8):
                                    ck = slice(i * 64, (i + 1) * 64)
                                    S.op(S.pe, ("matmul", dict(out=PS[py][0:64, ck], lhsT=AT[:, ck], rhs=A[:, ck], start=True, stop=True)), reads=[BA[cur], BAT[cur]], writes=[BPS[py]])
                            S.op(S.act, ("copy", dict(out=A2T[:], in_=PS[px][0:64, :])), reads=[BPS[px]], writes=[BAT[nxt]])
                            if lvl < 4:
                                S.op(S.dve, ("tensor_copy", dict(out=A2[:], in_=PS[py][0:64, :])), reads=[BPS[py]], writes=[BA[nxt]])
                            for i in range(8):
                                ck = slice(i * 64, (i + 1) * 64)
                                S.op(S.pe, ("matmul", dict(out=PS[pz][0:64, ck], lhsT=A2T[:, ck], rhs=Pm[:, ck], start=True, stop=True)), reads=[BAT[nxt], BP], writes=[BPS[pz]])
                            S.op(S.dve, ("tensor_tensor", dict(out=Pm[:], in0=PS[pz][0:64, :], in1=Pm[:], op=ALU.add)), reads=[BPS[pz], BP], writes=[BP])
                            cur = nxt
                        S.op(S.act, ("copy", dict(out=zto[gi][:], in_=Pm[:])), reads=[BP], writes=[Bzto[gi]])
                        S.dma(S.sp, ZT[(h, d)][cg], zto[gi][:], reads=[Bzto[gi]], writes=[BZT[(h, d)]])
          S.barrier()
        HQt = {}; HKt = {}; HQB = {}; HKD = {}
        BHQt = {}; BHKt = {}; BHQB = {}; BHKD = {}
        for h in range(n_h):
            for d in range(2):
                HQt[(h, d)] = scratch(f"HQt{h}{d}", [128, T], BF16); HKt[(h, d)] = scratch(f"HKt{h}{d}", [128, T], BF16)
                HQB[(h, d)] = scratch(f"HQB{h}{d}", [128, T], BF16); HKD[(h, d)] = scratch(f"HKD{h}{d}", [T, 128], BF16)
                BHQt[(h, d)] = Buf(); BHKt[(h, d)] = Buf(); BHQB[(h, d)] = Buf(); BHKD[(h, d)] = Buf()
                ALLOUT += [BHQt[(h, d)], BHKt[(h, d)], BHQB[(h, d)], BHKD[(h, d)]]
        if n_h:
          with contextlib.ExitStack() as ph:
            NL = n_h * 2
            lbr = S.sb("lbr", [128, NL * DEPTH_], F32, ph); Blb = Buf()
            S.dma(S.sp, lbr[:], hlb[:, :], writes=[Blb])
            lbv = lbr[:].rearrange("p (a l) -> p a l", l=DEPTH_)
            lw = S.sb("lw", [128, NL * 8], F32, ph)
            LW = lambda k: lw[:, k * NL:(k + 1) * NL]
            S.op(S.dve, ("tensor_copy", dict(out=LW(0), in_=lbv[:, :, 0])), reads=[Blb], writes=[Blb])
            for l in range(1, DEPTH_):
                S.op(S.dve, ("tensor_tensor", dict(out=LW(0), in0=LW(0), in1=lbv[:, :, l], op=ALU.max)), reads=[Blb], writes=[Blb])
            S.op(S.dve, ("tensor_tensor", dict(out=lbv, in0=lbv, in1=LW(0).unsqueeze(2).broadcast_to([128, NL, DEPTH_]), op=ALU.subtract)), reads=[Blb], writes=[Blb])
            S.op(S.act, ("activation", dict(out=lbr[:], in_=lbr[:], func=AF.Exp)), reads=[Blb], writes=[Blb])
            S.op(S.dve, ("tensor_reduce", dict(out=LW(1), in_=lbv, axis=AX.X, op=ALU.add)), reads=[Blb], writes=[Blb])
            S.op(S.dve, ("reciprocal", dict(out=LW(2), in_=LW(1))), reads=[Blb], writes=[Blb])
            S.op(S.dve, ("tensor_tensor", dict(out=lbv, in0=lbv, in1=LW(2).unsqueeze(2).broadcast_to([128, NL, DEPTH_]), op=ALU.mult)), reads=[Blb], writes=[Blb])
            cmk = S.sb("cmk", [128, DEPTH_], F32, ph)
            S.dma(S.sp, cmk[:], hcm[:, :], writes=[Blb])
            S.op(S.dve, ("tensor_scalar", dict(out=LW(3), in0=lbv[:, :, 0], scalar1=cmk[:, 0:1], scalar2=None, op0=ALU.mult)), reads=[Blb], writes=[Blb])
            for l in range(1, DEPTH_):
                S.op(S.dve, ("scalar_tensor_tensor", dict(out=LW(3), in0=lbv[:, :, l], scalar=cmk[:, l:l + 1], in1=LW(3), op0=ALU.mult, op1=ALU.add)), reads=[Blb], writes=[Blb])
            S.op(S.dve, ("tensor_tensor", dict(out=LW(4), in0=LW(3), in1=lbv[:, :, 0], op=ALU.subtract)), reads=[Blb], writes=[Blb])
            S.op(S.dve, ("tensor_scalar", dict(out=LW(5), in0=LW(4), scalar1=-1.0, scalar2=1.0, op0=ALU.mult, op1=ALU.add)), reads=[Blb], writes=[Blb])
            S.op(S.dve, ("tensor_scalar", dict(out=LW(6), in0=LW(5), scalar1=-1.0, scalar2=None, op0=ALU.mult)), reads=[Blb], writes=[Blb])
            xq = S.sb("hxq", [128, TB], F32, ph); Bxq = Buf()
            xf = S.sb("hxf", [128, TB], F32, ph); Bxf = Buf()
            sg_ = S.sb("hsg", [128, TB], F32, ph); Bsg = Buf()
            kin = S.sb("hkin", [128, TB], F32, ph); Bkin = Buf()
            ca = S.sb("hca", [128, TB], F32, ph); Bca = Buf()
            cb_ = S.sb("hcb", [128, TB], F32, ph); Bcb = Buf()
            w1 = S.sb("hw1", [128, TB], F32, ph); Bw1 = Buf()
            w2 = S.sb("hw2", [128, TB], F32, ph); Bw2 = Buf()
            hob = [S.sb(f"hob{i}", [128, TB], BF16, ph) for i in range(3)]; Bhob = [Buf(), Buf(), Buf()]
            tsb2 = [S.sb(f"htsb{i}", [128, 512], BF16, ph) for i in range(2)]; Btsb2 = [Buf(), Buf()]
            NCB = TB // 64
            c3 = lambda t_: t_[:].rearrange("p (n s) -> p n s", s=64)
            nts = 0
            for h in range(n_h):
                for d in range(2):
                    li = h * 2 + d
                    LB = lambda k: lw[:, k * NL + li:k * NL + li + 1]
                    for tb in range(NTB):
                        t0 = tb * TB
                        tsl = slice(t0, t0 + TB)
                        S.dma(S.sp, xq[:], FMs[f"hq{h}"][:, tsl], reads=[B_FM[f"hq{h}"]], writes=[Bxq])
                        S.dma(S.sp, xf[:], FMs[f"hf{d}{h}"][:, tsl], reads=[B_FM[f"hf{d}{h}"]], writes=[Bxf])
                        S.op(S.act, ("activation", dict(out=sg_[:], in_=xf[:], func=AF.Sigmoid)), reads=[Bxf], writes=[Bsg])
                        S.op(S.dve, ("tensor_scalar", dict(out=ca[:], in0=sg_[:], scalar1=LB(5), scalar2=LB(4), op0=ALU.mult, op1=ALU.add)), reads=[Bsg, Blb], writes=[Bca])
                        S.op(S.pool, ("tensor_scalar", dict(out=ca[:], in0=ca[:], scalar1=1.17549435e-38, scalar2=None, op0=ALU.max)), reads=[Bca], writes=[Bca])
                        S.op(S.act, ("activation", dict(out=ca[:], in_=ca[:], func=AF.Ln)), reads=[Bca], writes=[Bca])
                        S.op(S.dve, ("tensor_scalar", dict(out=kin[:], in0=sg_[:], scalar1=LB(6), scalar2=LB(5), op0=ALU.mult, op1=ALU.add)), reads=[Bsg, Blb], writes=[Bkin])
                        a, b, Ba, Bb = ca, cb_, Bca, Bcb
                        for sh in (1, 2, 4, 8, 16, 32):
                            if d == 0:
                                S.op(S.dve, ("tensor_tensor", dict(out=c3(b)[:, :, sh:], in0=c3(a)[:, :, sh:], in1=c3(a)[:, :, :64 - sh], op=ALU.add)), reads=[Ba], writes=[Bb])
                                S.op(S.pool, ("tensor_copy", dict(out=c3(b)[:, :, :sh], in_=c3(a)[:, :, :sh])), reads=[Ba], writes=[Bb])
                            else:
                                S.op(S.dve, ("tensor_tensor", dict(out=c3(b)[:, :, :64 - sh], in0=c3(a)[:, :, :64 - sh], in1=c3(a)[:, :, sh:], op=ALU.add)), reads=[Ba], writes=[Bb])
                                S.op(S.pool, ("tensor_copy", dict(out=c3(b)[:, :, 64 - sh:], in_=c3(a)[:, :, 64 - sh:])), reads=[Ba], writes=[Bb])
                            a, b, Ba, Bb = b, a, Bb, Ba
                        bc_, Bbc = a, Ba
                        lastc = 63 if d == 0 else 0
                        S.op(S.act, ("activation", dict(out=hsm[(h, d)]["el"][:, tb * NCB:(tb + 1) * NCB], in_=c3(bc_)[:, :, lastc], func=AF.Exp)), reads=[Bbc], writes=[hsm[(h, d)]["B"]])
                        S.op(S.dve, ("tensor_tensor", dict(out=c3(w1), in0=c3(bc_), in1=c3(bc_)[:, :, 32:33].broadcast_to([128, NCB, 64]), op=ALU.subtract)), reads=[Bbc], writes=[Bw1])
                        S.op(S.act, ("activation", dict(out=w2[:], in_=w1[:], func=AF.Exp)), reads=[Bw1], writes=[Bw2])
                        S.op(S.dve, ("tensor_tensor", dict(out=hob[0][:], in0=xq[:], in1=w2[:], op=ALU.mult)), reads=[Bxq, Bw2], writes=[Bhob[0]])
                        S.dma(S.sp, HQt[(h, d)][:, tsl], hob[0][:], reads=[Bhob[0]], writes=[BHQt[(h, d)]])
                        S.op(S.act, ("activation", dict(out=w2[:], in_=w1[:], func=AF.Exp, scale=-1.0)), reads=[Bw1, Bhob[0]], writes=[Bw2])
                        S.op(S.dve, ("tensor_tensor", dict(out=hob[1][:], in0=kin[:], in1=w2[:], op=ALU.mult)), reads=[Bkin, Bw2], writes=[Bhob[1]])
                        S.dma(S.sp, HKt[(h, d)][:, tsl], hob[1][:], reads=[Bhob[1]], writes=[BHKt[(h, d)]])
                        S.op(S.act, ("activation", dict(out=w2[:], in_=bc_[:], func=AF.Exp)), reads=[Bbc, Bhob[1]], writes=[Bw2])
                        S.op(S.dve, ("tensor_tensor", dict(out=hob[2][:], in0=xq[:], in1=w2[:], op=ALU.mult)), reads=[Bxq, Bw2], writes=[Bhob[2]])
                        S.dma(S.sp, HQB[(h, d)][:, tsl], hob[2][:], reads=[Bhob[2]], writes=[BHQB[(h, d)]])
                        S.op(S.dve, ("tensor_tensor", dict(out=c3(w1), in0=c3(bc_)[:, :, lastc:lastc + 1].broadcast_to([128, NCB, 64]), in1=c3(bc_), op=ALU.subtract)), reads=[Bbc, Bw1], writes=[Bw1])
                        S.op(S.act, ("activation", dict(out=w2[:], in_=w1[:], func=AF.Exp)), reads=[Bw1, Bhob[2]], writes=[Bw2])
                        S.op(S.dve, ("tensor_tensor", dict(out=w1[:], in0=kin[:], in1=w2[:], op=ALU.mult)), reads=[Bkin, Bw2], writes=[Bw1])
                        for sbk in range(TB // 512):
                            pb = nextpb()
                            for jj in range(4):
                                c0 = sbk * 512 + jj * 128
                                S.op(S.pe, ("transpose", dict(out=PS[pb][:, jj * 128:(jj + 1) * 128], in_=w1[:, c0:c0 + 128], identity=ident)), reads=[Bc, Bw1], writes=[BPS[pb]])
                            ts = nts % 2; nts += 1
                            tt0 = t0 + sbk * 512
                            S.op(S.act, ("copy", dict(out=tsb2[ts][:], in_=PS[pb][:, :])), reads=[BPS[pb]], writes=[Btsb2[ts]])
                            S.dma(S.sp, HKD[(h, d)][tt0:tt0 + 512, :].rearrange("(j p) e -> p j e", p=128), tsb2[ts][:].rearrange("p (j e) -> p j e", e=128),
                                  reads=[Btsb2[ts]], writes=[BHKD[(h, d)]])
          S.barrier()
        OH = {}; BOH = {}
        for h in range(n_h):
            for d in range(2):
                OH[(h, d)] = scratch(f"OH{h}{d}", [T, 128], F32); BOH[(h, d)] = Buf()
        with contextlib.ExitStack() as ph:
            scans = []
            bank = [0]
            def gdn_scan(h, d, tag):
                sm = gsm[(h, d)]
                pA = bank[0]; pB = bank[0] + 1; bank[0] += 2
                Sf = S.sb(f"Sf{tag}", [128, 128], F32, ph); BSf = Buf()
                Sb = S.sb(f"Sb{tag}", [128, 128], BF16, ph); BSb = Buf()
                kg2 = [S.sb(f"dk{tag}{i}", [128, 512], BF16, ph) for i in range(2)]
                qg2 = [S.sb(f"dq{tag}{i}", [128, 512], BF16, ph) for i in range(2)]
                kt2 = [S.sb(f"dkt{tag}{i}", [64, 8 * 128], BF16, ph) for i in range(2)]
                vt2 = [S.sb(f"dvt{tag}{i}", [64, 8 * 128], F32, ph) for i in range(2)]
                zt2 = [S.sb(f"dz{tag}{i}", [64, 512], BF16, ph) for i in range(2)]
                qk2 = [S.sb(f"dqk{tag}{i}", [64, 512], BF16, ph) for i in range(2)]
                og2 = [S.sb(f"dog{tag}{i}", [64, 8 * 128], F32, ph) for i in range(2)]
                Bin = [Buf(), Buf()]; Bog = [Buf(), Buf()]
                Rb = S.sb(f"dR{tag}", [64, 128], BF16, ph); BR = Buf()
                vn = S.sb(f"dvn{tag}", [64, 128], BF16, ph); Bvn = Buf()
                vs = S.sb(f"dvs{tag}", [64, 128], BF16, ph); Bvs = Buf()
                S.op(S.pool, ("memset", dict(ap=Sf[:], constant=0.0)), writes=[BSf])
                S.op(S.pool, ("memset", dict(ap=Sb[:], constant=0.0)), writes=[BSb])
                yield
                groups = list(range(NG)) if d == 0 else list(range(NG - 1, -1, -1))
                for gidx, cg in enumerate(groups):
                    gi = gidx % 2
                    tk = slice(cg * 512, (cg + 1) * 512)
                    S.dma(S.sp, kg2[gi][:], KhT[h][:, tk], reads=[BKhT[h]], writes=[Bin[gi]])
                    S.dma(S.sp, qg2[gi][:], QG[(h, d)][:, tk], reads=[BQG[(h, d)]], writes=[Bin[gi]])
                    S.dma(S.sp, kt2[gi][:].rearrange("p (i e) -> p i e", e=128), Ktok[h][tk, :].rearrange("(i s) e -> s i e", s=64), reads=[BKtok[h]], writes=[Bin[gi]])
                    S.dma(S.sp, vt2[gi][:].rearrange("p (i e) -> p i e", e=128), Vtok[h][tk, :].rearrange("(i s) e -> s i e", s=64), reads=[BVtok[h]], writes=[Bin[gi]])
                    S.dma(S.sp, zt2[gi][:], ZT[(h, d)][cg], reads=[BZT[(h, d)]], writes=[Bin[gi]])
                    S.dma(S.sp, qk2[gi][:], QKM[(h, d)][cg], reads=[BQKM[(h, d)]], writes=[Bin[gi]])
                    yield
                    order = range(8) if d == 0 else range(7, -1, -1)
                    for i in order:
                        n = cg * 8 + i
                        ck = slice(i * 64, (i + 1) * 64)
                        ce = slice(i * 128, (i + 1) * 128)
                        S.op(S.pe, ("matmul", dict(out=PS[pA][0:64, 0:128], lhsT=kg2[gi][:, ck], rhs=Sb[:], start=True, stop=True)), reads=[Bin[gi], BSb], writes=[BPS[pA]])
                        S.op(S.dve, ("scalar_tensor_tensor", dict(out=Rb[:], in0=PS[pA][0:64, 0:128], scalar=sm["neg"][:, n:n + 1], in1=vt2[gi][:, ce], op0=ALU.mult, op1=ALU.add)),
                             reads=[BPS[pA], sm["B"], Bin[gi]], writes=[BR])
                        yield
                        S.op(S.pe, ("matmul", dict(out=PS[pA][0:64, 128:256], lhsT=zt2[gi][:, ck], rhs=Rb[:], start=True, stop=True)), reads=[Bin[gi], BR], writes=[BPS[pA]])
                        S.op(S.act, ("activation", dict(out=vn[:], in_=PS[pA][0:64, 128:256], func=AF.Copy, scale=sm["b"][:, n:n + 1])), reads=[BPS[pA], sm["B"]], writes=[Bvn])
                        S.op(S.dve, ("tensor_scalar", dict(out=vs[:], in0=PS[pA][0:64, 128:256], scalar1=sm["bkd"][:, n:n + 1], scalar2=None, op0=ALU.mult)), reads=[BPS[pA], sm["B"]], writes=[Bvs])
                        yield
                        S.op(S.pe, ("matmul", dict(out=PS[pB][0:64, 0:128], lhsT=qg2[gi][:, ck], rhs=Sb[:], start=True, stop=False)), reads=[Bin[gi], BSb], writes=[BPS[pB]])
                        S.op(S.pe, ("matmul", dict(out=PS[pB][0:64, 0:128], lhsT=qk2[gi][:, ck], rhs=vn[:], start=False, stop=True)), reads=[Bin[gi], Bvn], writes=[BPS[pB]])
                        S.op(S.act, ("copy", dict(out=og2[gi][:, ce], in_=PS[pB][0:64, 0:128])), reads=[BPS[pB]], writes=[Bog[gi]])
                        S.op(S.pe, ("matmul", dict(out=PS[pB][:, 128:256], lhsT=kt2[gi][:, ce], rhs=vs[:], start=True, stop=True)), reads=[Bin[gi], Bvs], writes=[BPS[pB]])
                        S.op(S.dve, ("scalar_tensor_tensor", dict(out=Sf[:], in0=Sf[:], scalar=sm["gl"][:, n:n + 1], in1=PS[pB][:, 128:256], op0=ALU.mult, op1=ALU.add)),
                             reads=[BSf, sm["B"], BPS[pB]], writes=[BSf])
                        S.op(S.act, ("copy", dict(out=Sb[:], in_=Sf[:])), reads=[BSf], writes=[BSb])
                        yield
                    S.dma(S.sp, OG[(h, d)][tk, :].rearrange("(i s) e -> s i e", s=64), og2[gi][:].rearrange("p (i e) -> p i e", e=128), reads=[Bog[gi]], writes=[BOG[(h, d)]])
                    yield

            def hgrn_scan(h, d, tag):
                sm = hsm[(h, d)]
                pA = bank[0]; pB = bank[0] + 1; bank[0] += 2
                vc = n_g * 128 + h * 256
                Sf = S.sb(f"Sf{tag}", [128, 128], F32, ph); BSf = Buf()
                Sb = S.sb(f"Sb{tag}", [128, 128], BF16, ph); BSb = Buf()
                qt2 = [S.sb(f"hq{tag}{i}", [128, 512], BF16, ph) for i in range(2)]
                kt2 = [S.sb(f"hk{tag}{i}", [128, 512], BF16, ph) for i in range(2)]
                qb2 = [S.sb(f"hb{tag}{i}", [128, 512], BF16, ph) for i in range(2)]
                kd2 = [S.sb(f"hd{tag}{i}", [64, 8 * 128], BF16, ph) for i in range(2)]
                v2 = [S.sb(f"hv{tag}{i}", [64, 8 * 128], BF16, ph) for i in range(2)]
                og2 = [S.sb(f"ho{tag}{i}", [64, 8 * 128], F32, ph) for i in range(2)]
                am2 = [S.sb(f"ha{tag}{i}", [64, 512], BF16, ph) for i in range(2)]
                Bin = [Buf(), Buf()]; Bog = [Buf(), Buf()]; Bam = [Buf(), Buf()]
                S.op(S.pool, ("memset", dict(ap=Sf[:], constant=0.0)), writes=[BSf])
                S.op(S.pool, ("memset", dict(ap=Sb[:], constant=0.0)), writes=[BSb])
                yield
                groups = list(range(NG)) if d == 0 else list(range(NG - 1, -1, -1))
                for gidx, cg in enumerate(groups):
                    gi = gidx % 2
                    tk = slice(cg * 512, (cg + 1) * 512)
                    S.dma(S.sp, qt2[gi][:], HQt[(h, d)][:, tk], reads=[BHQt[(h, d)]], writes=[Bin[gi]])
                    S.dma(S.sp, kt2[gi][:], HKt[(h, d)][:, tk], reads=[BHKt[(h, d)]], writes=[Bin[gi]])
                    S.dma(S.sp, qb2[gi][:], HQB[(h, d)][:, tk], reads=[BHQB[(h, d)]], writes=[Bin[gi]])
                    S.dma(S.sp, kd2[gi][:].rearrange("p (i e) -> p i e", e=128), HKD[(h, d)][tk, :].rearrange("(i s) e -> s i e", s=64), reads=[BHKD[(h, d)]], writes=[Bin[gi]])
                    S.dma(S.pool, v2[gi][:].rearrange("p (i e) -> p i e", e=128), TMs[tk, vc:vc + 128].rearrange("(i s) e -> s i e", s=64), reads=[B_TM], writes=[Bin[gi]])
                    yield
                    for i in range(8):
                        ck = slice(i * 64, (i + 1) * 64)
                        S.op(S.pe, ("matmul", dict(out=PS[pA][0:64, ck], lhsT=kt2[gi][:, ck], rhs=qt2[gi][:, ck], start=True, stop=True)), reads=[Bin[gi]], writes=[BPS[pA]])
                    S.op(S.dve, ("tensor_tensor", dict(out=v8(am2[gi][:]), in0=v8(PS[pA][0:64, :]), in1=bc8(TRI(d)), op=ALU.mult)), reads=[BPS[pA], Bc], writes=[Bam[gi]])
                    yield
                    order = range(8) if d == 0 else range(7, -1, -1)
                    for i in order:
                        n = cg * 8 + i
                        ck = slice(i * 64, (i + 1) * 64)
                        ce = slice(i * 128, (i + 1) * 128)
                        S.op(S.pe, ("matmul", dict(out=PS[pB][0:64, 0:128], lhsT=qb2[gi][:, ck], rhs=Sb[:], start=True, stop=False)), reads=[Bin[gi], BSb], writes=[BPS[pB]])
                        S.op(S.pe, ("matmul", dict(out=PS[pB][0:64, 0:128], lhsT=am2[gi][:, ck], rhs=v2[gi][:, ce], start=False, stop=True)), reads=[Bin[gi], Bam[gi]], writes=[BPS[pB]])
                        S.op(S.act, ("copy", dict(out=og2[gi][:, ce], in_=PS[pB][0:64, 0:128])), reads=[BPS[pB]], writes=[Bog[gi]])
                        S.op(S.pe, ("matmul", dict(out=PS[pB][:, 128:256], lhsT=kd2[gi][:, ce], rhs=v2[gi][:, ce], start=True, stop=True)), reads=[Bin[gi]], writes=[BPS[pB]])
                        S.op(S.dve, ("scalar_tensor_tensor", dict(out=Sf[:], in0=Sf[:], scalar=sm["el"][:, n:n + 1], in1=PS[pB][:, 128:256], op0=ALU.mult, op1=ALU.add)),
                             reads=[BSf, sm["B"], BPS[pB]], writes=[BSf])
                        S.op(S.act, ("copy", dict(out=Sb[:], in_=Sf[:])), reads=[BSf], writes=[BSb])
                        yield
                    S.dma(S.sp, OH[(h, d)][tk, :].rearrange("(i s) e -> s i e", s=64), og2[gi][:].rearrange("p (i e) -> p i e", e=128), reads=[Bog[gi]], writes=[BOH[(h, d)]])
                    yield

            def run_scans(scans):
                live = list(scans)
                while live:
                    nxt_live = []
                    for gnr in live:
                        try:
                            next(gnr)
                            nxt_live.append(gnr)
                        except StopIteration:
                            pass
                    live = nxt_live
            run_scans([gdn_scan(h, d, f"g{h}{d}") for h in range(n_g) for d in range(2)])
        S.barrier()
        with contextlib.ExitStack() as ph:
            bank[0] = 0
            run_scans([hgrn_scan(h, d, f"h{h}{d}") for h in range(n_h) for d in range(2)])
        S.barrier()
        NOUT = (n_g + n_h) * 128
        omix = nc.dram_tensor("omix", [T, NOUT], BF16, kind="ExternalOutput").ap()
        Bomix = Buf(); ALLOUT.append(Bomix)
        with contextlib.ExitStack() as ph:
            nwt = S.sb("nwt", [128, 256], F32, ph); Bnw = Buf()
            S.dma(S.sp, nwt[:], normw[:, :], writes=[Bnw])
            JT = min(8, T // 128)
            of_ = [S.sb(f"e_of{i}", [128, JT * 128], F32, ph) for i in range(2)]
            ob_ = [S.sb(f"e_ob{i}", [128, JT * 128], F32, ph) for i in range(2)]
            zz_ = [S.sb(f"e_z{i}", [128, JT * 128], F32, ph) for i in range(2)]
            Bei = [Buf(), Buf()]
            sqe = S.sb("e_sq", [128, JT * 128], F32, ph); Bsqe = Buf()
            sse = S.sb("e_ss", [128, JT], F32, ph); Bsse = Buf()
            yo = [S.sb(f"e_y{i}", [128, JT * 128], BF16, ph) for i in range(2)]; Byo = [Buf(), Buf()]
            ne = 0
            jobs = [("g", h) for h in range(n_g)] + [("h", h) for h in range(n_h)]
            for ji, (kind, h) in enumerate(jobs):
                if kind == "g":
                    Of, Ob, BOf, BOb = OG[(h, 0)], OG[(h, 1)], BOG[(h, 0)], BOG[(h, 1)]
                    zc = h * 128; wcol = 0
                else:
                    Of, Ob, BOf, BOb = OH[(h, 0)], OH[(h, 1)], BOH[(h, 0)], BOH[(h, 1)]
                    zc = n_g * 128 + h * 256 + 128; wcol = 128
                v3 = lambda t_: t_[:].rearrange("p (j e) -> p j e", e=128)
                for t0 in range(0, T, JT * 128):
                    e = ne % 2; ne += 1
                    tsl = slice(t0, t0 + JT * 128)
                    S.dma(S.sp, v3(of_[e]), Of[tsl, :].rearrange("(j p) e -> p j e", p=128), reads=[BOf], writes=[Bei[e]])
                    S.dma(S.sp, v3(ob_[e]), Ob[tsl, :].rearrange("(j p) e -> p j e", p=128), reads=[BOb], writes=[Bei[e]])
                    S.dma(S.sp, v3(zz_[e]), TMs[tsl, zc:zc + 128].rearrange("(j p) e -> p j e", p=128), reads=[B_TM], writes=[Bei[e]])
                    S.op(S.dve, ("tensor_tensor", dict(out=of_[e][:], in0=of_[e][:], in1=ob_[e][:], op=ALU.add)), reads=[Bei[e]], writes=[Bei[e]])
                    S.op(S.act, ("activation", dict(out=sqe[:], in_=of_[e][:], func=AF.Square)), reads=[Bei[e]], writes=[Bsqe])
                    S.op(S.dve, ("tensor_reduce", dict(out=sse[:], in_=v3(sqe), axis=AX.X, op=ALU.add)), reads=[Bsqe], writes=[Bsse])
                    S.op(S.dve, ("tensor_scalar", dict(out=sse[:], in0=sse[:], scalar1=1.0 / 128, scalar2=1e-6, op0=ALU.mult, op1=ALU.add)), reads=[Bsse], writes=[Bsse])
                    S.op(S.act, ("sqrt", dict(out=sse[:], in_=sse[:])), reads=[Bsse], writes=[Bsse])
                    S.op(S.dve, ("reciprocal", dict(out=sse[:], in_=sse[:])), reads=[Bsse], writes=[Bsse])
                    S.op(S.dve, ("tensor_tensor", dict(out=v3(of_[e]), in0=v3(of_[e]), in1=sse[:].unsqueeze(2).broadcast_to([128, JT, 128]), op=ALU.mult)), reads=[Bei[e], Bsse], writes=[Bei[e]])
                    S.op(S.pool, ("tensor_tensor", dict(out=v3(of_[e]), in0=v3(of_[e]), in1=nwt[:, wcol:wcol + 128].unsqueeze(1).broadcast_to([128, JT, 128]), op=ALU.mult)), reads=[Bei[e], Bnw], writes=[Bei[e]])
                    S.op(S.act, ("activation", dict(out=zz_[e][:], in_=zz_[e][:], func=AF.Silu)), reads=[Bei[e]], writes=[Bei[e]])
                    S.op(S.dve, ("tensor_tensor", dict(out=yo[e][:], in0=of_[e][:], in1=zz_[e][:], op=ALU.mult)), reads=[Bei[e]], writes=[Byo[e]])
                    S.dma(S.sp, omix[tsl, ji * 128:(ji + 1) * 128].rearrange("(j p) e -> p j e", p=128), v3(yo[e]), reads=[Byo[e]], writes=[Bomix])

        S.finish([b for b in ALLOUT])
        S.emit()
    return nc, dbg


D = 2048
KC = 16
NE = 16


def build_tok(Tc, mode, KCH=0, last=False):
    nc = bass.Bass("TRN2", target_bir_lowering=False)
    NT = Tc // 128
    h_in = nc.dram_tensor("h", [Tc, D], F32, kind="ExternalInput").ap()
    nwb = nc.dram_tensor("nwb", [128, D], F32, kind="ExternalInput").ap()
    if mode == "O":
        oT = nc.dram_tensor("oT", [D, Tc], BF16, kind="ExternalInput").ap()
        w_out = nc.dram_tensor("w_out", [D, D], F32, kind="ExternalInput").ap()
        w_r = nc.dram_tensor("w_r", [D, NE], F32, kind="ExternalInput").ap()
        ident_in = nc.dram_tensor("ident", [128, 128], F32, kind="ExternalInput").ap()
        u_out = nc.dram_tensor("u_out", [Tc, D], BF16, kind="ExternalOutput").ap()
        p_out = nc.dram_tensor("p_out", [Tc, NE], F32, kind="ExternalOutput").ap()
    elif mode == "C":
        oT = nc.dram_tensor("oT", [D, Tc], BF16, kind="ExternalInput").ap()
        w_out = nc.dram_tensor("w_out", [D, D], F32, kind="ExternalInput").ap()
        rows = nc.dram_tensor("rows", [NT, max(KCH, 1), 128, D], BF16, kind="ExternalInput").ap()
        idx = nc.dram_tensor("idx", [128, NT * max(KCH, 1)], F32, kind="ExternalInput").ap()
        iota_in = nc.dram_tensor("iota", [128, 128], F32, kind="ExternalInput").ap()
        if last:
            f_out = nc.dram_tensor("f_out", [Tc, D], F32, kind="ExternalOutput").ap()
        else:
            h_out = nc.dram_tensor("h_out", [Tc, D], F32, kind="ExternalOutput").ap()
            u_out = nc.dram_tensor("u_out", [Tc, D], BF16, kind="ExternalOutput").ap()
    else:
        u_out = nc.dram_tensor("u_out", [Tc, D], BF16, kind="ExternalOutput").ap()
    with contextlib.ExitStack() as st:
        S = Sched(nc, st)
        PS = [S.ps(f"ps{i}", [128, 512]) for i in range(8)]
        BPS = [Buf(f"ps{i}", excl=True) for i in range(8)]
        Bout = Buf()
        nw = S.sb("nw", [128, D]); Bnw = Buf()
        S.dma(S.sp, nw[:], nwb[:, :], writes=[Bnw])
        ht = [S.sb(f"ht{i}", [128, D]) for i in range(2)]; Bht = [Buf(), Buf()]
        junk = S.sb("junk", [128, D]); Bj = Buf()
        ss = [S.sb(f"ss{i}", [128, 8]) for i in range(2)]; Bss = [Buf(), Buf()]
        if mode == "O":
            wo = S.sb("wo", [128, KC, D], BF16); Bwo = Buf()
            for kc in range(KC):
                S.dma(S.pool, wo[:, kc, :], w_out[kc * 128:(kc + 1) * 128, :], writes=[Bwo])
            wr = S.sb("wr", [128, KC, NE]); Bwr = Buf()
            S.dma(S.sp, wr[:], w_r.rearrange("(kc p) e -> p kc e", p=128), writes=[Bwr])
            idt = S.sb("idt", [128, 128]); Bid = Buf()
            S.dma(S.sp, idt[:], ident_in[:, :], writes=[Bid])
            ot = [S.sb(f"ot{i}", [128, KC, 512], BF16) for i in range(2)]; Bot = [Buf(), Buf()]
            uf = S.sb("uf", [128, D]); Buf_ = Buf()
            uT = S.sb("uT", [128, KC, 128]); BuT = Buf()
            lg = [S.sb(f"lg{i}", [128, 32]) for i in range(2)]; Blg = [Buf(), Buf()]
            oTv = oT.rearrange("(kc p) t -> p kc t", p=128)
        if mode == "C":
            wo = S.sb("wo", [128, KC, D], BF16); Bwo = Buf()
            for kc in range(KC):
                S.dma(S.pool, wo[:, kc, :], w_out[kc * 128:(kc + 1) * 128, :], writes=[Bwo])
            ot = [S.sb(f"ot{i}", [128, KC, 512], BF16) for i in range(2)]; Bot = [Buf(), Buf()]
            oTv = oT.rearrange("(kc p) t -> p kc t", p=128)
            io = S.sb("io", [128, 128]); Bio = Buf()
            S.dma(S.sp, io[:], iota_in[:, :], writes=[Bio])
            ix = S.sb("ix", [128, NT * max(KCH, 1)]); Bix = Buf()
            S.dma(S.sp, ix[:], idx[:, :], writes=[Bix])
            rw = [S.sb(f"rw{i}", [128, D], BF16) for i in range(3)]; Brw = [Buf(), Buf(), Buf()]
            Pm = [S.sb(f"Pm{i}", [128, 128], BF16) for i in range(3)]; BPm = [Buf(), Buf(), Buf()]
        ub = [S.sb(f"ub{i}", [128, D], BF16) for i in range(2)]; Bub = [Buf(), Buf()]
        fo = S.sb("fo", [128, D]) if (mode == "C" and last) else None; Bfo = Buf()
        nrw = 0
        for i in range(NT):
            k = i % 2
            tsl = slice(i * 128, (i + 1) * 128)
            S.dma(S.sp, ht[k][:], h_in[tsl, :], writes=[Bht[k]])
            if mode == "O":
                if i % 4 == 0:
                    ob = (i // 4) % 2
                    S.dma(S.sp, ot[ob][:], oTv[:, :, i * 128:i * 128 + 512], writes=[Bot[ob]])
                tt = i % 4
                for nb in range(4):
                    for kc in range(KC):
                        S.op(S.pe, ("matmul", dict(out=PS[nb][:, :], lhsT=ot[ob][:, kc, tt * 128:(tt + 1) * 128], rhs=wo[:, kc, nb * 512:(nb + 1) * 512],
                                                   start=(kc == 0), stop=(kc == KC - 1))), reads=[Bot[ob], Bwo], writes=[BPS[nb]])
                for nb in range(4):
                    sl = slice(nb * 512, (nb + 1) * 512)
                    S.op(S.dve, ("tensor_tensor", dict(out=ht[k][:, sl], in0=ht[k][:, sl], in1=PS[nb][:, :], op=ALU.add)), reads=[BPS[nb], Bht[k]], writes=[Bht[k]])
            if mode == "C":
                if True:
                    if i % 4 == 0:
                        ob = (i // 4) % 2
                        S.dma(S.sp, ot[ob][:], oTv[:, :, i * 128:i * 128 + 512], writes=[Bot[ob]])
                    tt = i % 4
                    rws = []
                    for j in range(KCH):
                        r = nrw % 3; nrw += 1
                        S.dma(S.sp if j % 2 == 0 else S.act, rw[r][:], rows[i, j], writes=[Brw[r]])
                        S.op(S.dve, ("tensor_scalar", dict(out=Pm[r][:], in0=io[:], scalar1=ix[:, i * KCH + j:i * KCH + j + 1], scalar2=None, op0=ALU.is_equal)),
                             reads=[Bio, Bix], writes=[BPm[r]])
                        rws.append(r)
                    for nb in range(4):
                        for kc in range(KC):
                            S.op(S.pe, ("matmul", dict(out=PS[nb][:, :], lhsT=ot[ob][:, kc, tt * 128:(tt + 1) * 128], rhs=wo[:, kc, nb * 512:(nb + 1) * 512],
                                                       start=(kc == 0), stop=(kc == KC - 1 and KCH == 0))), reads=[Bot[ob], Bwo], writes=[BPS[nb]])
                        for j, r in enumerate(rws):
                            S.op(S.pe, ("matmul", dict(out=PS[nb][:, :], lhsT=Pm[r][:], rhs=rw[r][:, nb * 512:(nb + 1) * 512], start=False, stop=(j == KCH - 1))),
                                 reads=[BPm[r], Brw[r]], writes=[BPS[nb]])
                    for nb in range(4):
                        sl = slice(nb * 512, (nb + 1) * 512)
                        S.op(S.dve, ("tensor_tensor", dict(out=ht[k][:, sl], in0=ht[k][:, sl], in1=PS[nb][:, :], op=ALU.add)), reads=[BPS[nb], Bht[k]], writes=[Bht[k]])
                if not last:
                    S.dma(S.sp, h_out[tsl, :], ht[k][:], reads=[Bht[k]], writes=[Bout])
            S.op(S.act, ("activation", dict(out=junk[:], in_=ht[k][:], func=AF.Square, accum_out=ss[k][:, 0:1])), reads=[Bht[k]], writes=[Bj, Bss[k]])
            S.op(S.dve, ("tensor_scalar", dict(out=ss[k][:, 1:2], in0=ss[k][:, 0:1], scalar1=1.0 / D, scalar2=1e-6, op0=ALU.mult, op1=ALU.add)), reads=[Bss[k]], writes=[Bss[k]])
            S.op(S.act, ("sqrt", dict(out=ss[k][:, 2:3], in_=ss[k][:, 1:2])), reads=[Bss[k]], writes=[Bss[k]])
            S.op(S.dve, ("reciprocal", dict(out=ss[k][:, 3:4], in_=ss[k][:, 2:3])), reads=[Bss[k]], writes=[Bss[k]])
            if mode == "C" and last:
                S.op(S.dve, ("scalar_tensor_tensor", dict(out=fo[:], in0=ht[k][:], scalar=ss[k][:, 3:4], in1=nw[:], op0=ALU.mult, op1=ALU.mult)), reads=[Bht[k], Bss[k], Bnw], writes=[Bfo])
                S.dma(S.sp, f_out[tsl, :], fo[:], reads=[Bfo], writes=[Bout])
            elif mode == "O":
                S.op(S.dve, ("scalar_tensor_tensor", dict(out=uf[:], in0=ht[k][:], scalar=ss[k][:, 3:4], in1=nw[:], op0=ALU.mult, op1=ALU.mult)), reads=[Bht[k], Bss[k], Bnw], writes=[Buf_])
                S.op(S.act, ("copy", dict(out=ub[k][:], in_=uf[:])), reads=[Buf_], writes=[Bub[k]])
                S.dma(S.sp, u_out[tsl, :], ub[k][:], reads=[Bub[k]], writes=[Bout])
                for q4 in range(4):
                    pb = 4 + q4
                    for jj in range(4):
                        kc = q4 * 4 + jj
                        S.op(S.pe, ("transpose", dict(out=PS[pb][:, jj * 128:(jj + 1) * 128], in_=uf[:, kc * 128:(kc + 1) * 128], identity=idt[:])), reads=[Buf_, Bid], writes=[BPS[pb]])
                    dst = uT[:, q4 * 4:(q4 + 1) * 4, :]
                    srcv = PS[pb][:, :].rearrange("p (a b) -> p a b", b=128)
                    if q4 % 2 == 0:
                        S.op(S.act, ("copy", dict(out=dst, in_=srcv)), reads=[BPS[pb]], writes=[BuT])
                    else:
                        S.op(S.dve, ("tensor_copy", dict(out=dst, in_=srcv)), reads=[BPS[pb]], writes=[BuT])
                for kc in range(KC):
                    S.op(S.pe, ("matmul", dict(out=PS[4][:, 0:NE], lhsT=uT[:, kc, :], rhs=wr[:, kc, :], start=(kc == 0), stop=(kc == KC - 1))), reads=[BuT, Bwr], writes=[BPS[4]])
                l = lg[k]
                S.op(S.dve, ("tensor_reduce", dict(out=l[:, 16:17], in_=PS[4][:, 0:NE], axis=AX.X, op=ALU.max)), reads=[BPS[4]], writes=[Blg[k]])
                S.op(S.dve, ("tensor_scalar", dict(out=l[:, 17:18], in0=l[:, 16:17], scalar1=-1.0, scalar2=None, op0=ALU.mult)), reads=[Blg[k]], writes=[Blg[k]])
                S.op(S.act, ("activation", dict(out=l[:, 0:NE], in_=PS[4][:, 0:NE], func=AF.Exp, bias=l[:, 17:18], accum_out=l[:, 18:19])), reads=[BPS[4], Blg[k]], writes=[Blg[k]])
                S.op(S.dve, ("reciprocal", dict(out=l[:, 19:20], in_=l[:, 18:19])), reads=[Blg[k]], writes=[Blg[k]])
                S.op(S.dve, ("tensor_scalar", dict(out=l[:, 0:NE], in0=l[:, 0:NE], scalar1=l[:, 19:20], scalar2=None, op0=ALU.mult)), reads=[Blg[k]], writes=[Blg[k]])
                S.dma(S.sp, p_out[tsl, :], l[:, 0:NE], reads=[Blg[k]], writes=[Bout])
            else:
                S.op(S.dve, ("scalar_tensor_tensor", dict(out=ub[k][:], in0=ht[k][:], scalar=ss[k][:, 3:4], in1=nw[:], op0=ALU.mult, op1=ALU.mult)), reads=[Bht[k], Bss[k], Bnw], writes=[Bub[k]])
                S.dma(S.sp, u_out[tsl, :], ub[k][:], reads=[Bub[k]], writes=[Bout])
        S.finish([Bout])
        S.emit()
    return nc


D = 2048
KC = 16


def build_topk(NBE, J, KCAP, iters=36):
    nc = bass.Bass("TRN2", target_bir_lowering=False)
    P_in = nc.dram_tensor("P", [128, NBE * J], F32, kind="ExternalInput").ap()
    ones_in = nc.dram_tensor("ones", [128, 128], F32, kind="ExternalInput").ap()
    m_out = nc.dram_tensor("mask", [128, NBE * J], F32, kind="ExternalOutput").ap()
    c_out = nc.dram_tensor("cnt", [128, NBE], F32, kind="ExternalOutput").ap()
    with contextlib.ExitStack() as st:
        S = Sched(nc, st)
        PS = [S.ps(f"ps{i}", [128, 512]) for i in range(2)]
        BPS = [Buf(f"ps{i}", excl=True) for i in range(2)]
        P = S.sb("Psb", [128, NBE * J]); BP = Buf()
        S.dma(S.sp, P[:], P_in[:, :], writes=[BP])
        on = S.sb("on", [128, 128]); Bon = Buf()
        S.dma(S.sp, on[:], ones_in[:, :], writes=[Bon])
        cmp_ = S.sb("cmp", [128, NBE * J]); Bcmp = Buf()
        w = S.sb("w", [128, 8 * NBE]); Bw = Buf()
        W = lambda k: w[:, k * NBE:(k + 1) * NBE]
        lo, hi, mid, cnt, ge, t1, t2 = (W(k) for k in range(7))
        P3 = P[:].rearrange("p (a j) -> p a j", j=J)
        C3 = cmp_[:].rearrange("p (a j) -> p a j", j=J)
        S.op(S.pool, ("memset", dict(ap=lo, constant=0.0)), writes=[Bw])
        S.op(S.pool, ("memset", dict(ap=hi, constant=2.0)), writes=[Bw])
        def count(thr, pb):
            S.op(S.dve, ("tensor_tensor", dict(out=C3, in0=P3, in1=thr.unsqueeze(2).broadcast_to([128, NBE, J]), op=ALU.is_ge)), reads=[BP, Bw], writes=[Bcmp])
            S.op(S.dve, ("tensor_reduce", dict(out=cnt, in_=C3, axis=AX.X, op=ALU.add)), reads=[Bcmp], writes=[Bw])
            S.op(S.pe, ("matmul", dict(out=PS[pb][:, 0:NBE], lhsT=on[:], rhs=cnt, start=True, stop=True)), reads=[Bon, Bw], writes=[BPS[pb]])
        for it in range(iters):
            pb = it % 2
            S.op(S.dve, ("tensor_tensor", dict(out=mid, in0=lo, in1=hi, op=ALU.add)), reads=[Bw], writes=[Bw])
            S.op(S.dve, ("tensor_scalar", dict(out=mid, in0=mid, scalar1=0.5, scalar2=None, op0=ALU.mult)), reads=[Bw], writes=[Bw])
            count(mid, pb)
            S.op(S.dve, ("tensor_scalar", dict(out=ge, in0=PS[pb][:, 0:NBE], scalar1=float(KCAP) - 0.5, scalar2=None, op0=ALU.is_ge)), reads=[BPS[pb]], writes=[Bw])
            S.op(S.dve, ("tensor_tensor", dict(out=t1, in0=ge, in1=mid, op=ALU.mult)), reads=[Bw], writes=[Bw])
            S.op(S.dve, ("tensor_tensor", dict(out=lo, in0=lo, in1=t1, op=ALU.max)), reads=[Bw], writes=[Bw])
            S.op(S.dve, ("scalar_tensor_tensor", dict(out=t2, in0=ge, scalar=4.0, in1=mid, op0=ALU.mult, op1=ALU.add)), reads=[Bw], writes=[Bw])
            S.op(S.dve, ("tensor_tensor", dict(out=hi, in0=hi, in1=t2, op=ALU.min)), reads=[Bw], writes=[Bw])
        count(lo, 0)
        S.op(S.dve, ("tensor_copy", dict(out=t1, in_=PS[0][:, 0:NBE])), reads=[BPS[0]], writes=[Bw])
        Bo = Buf()
        S.dma(S.sp, m_out[:, :], cmp_[:], reads=[Bcmp], writes=[Bo])
        S.dma(S.sp, c_out[:, :], t1, reads=[Bw], writes=[Bo])
        S.finish([Bo])
        S.emit()
    return nc


def build_experts(NJ, CAP, FF=2048):
    nc = bass.Bass("TRN2", target_bir_lowering=False)
    NW = 2
    JPW = NJ // NW
    NTT = CAP // 128
    NTB = CAP // 512
    FC = FF // 128
    XT = nc.dram_tensor("XT", [NJ, D, CAP], BF16, kind="ExternalInput").ap()
    gates = nc.dram_tensor("gates", [128, NJ * NTT], F32, kind="ExternalInput").ap()
    wg = nc.dram_tensor("wg", [NW, D, FF], F32, kind="ExternalInput").ap()
    wu = nc.dram_tensor("wu", [NW, D, FF], F32, kind="ExternalInput").ap()
    wd = nc.dram_tensor("wd", [NW, FF, D], F32, kind="ExternalInput").ap()
    Y = nc.dram_tensor("Y", [NJ, CAP, D], BF16, kind="ExternalOutput").ap()
    with contextlib.ExitStack() as st:
        S = Sched(nc, st)
        PS = [S.ps(f"ps{i}", [128, 512]) for i in range(8)]
        BPS = [Buf(f"ps{i}", excl=True) for i in range(8)]
        gt = S.sb("gt", [128, NJ * NTT]); Bgt = Buf()
        S.dma(S.sp, gt[:], gates[:, :], writes=[Bgt])
        xw = S.sb("xw", [128, KC * max(CAP, D)], BF16); Bxw = Buf()
        hid = S.sb("hid", [128, FC * CAP], BF16); Bhid = Buf()
        wgs = [S.sb(f"wgs{i}", [128, KC * 128], BF16) for i in range(2)]; Bwgs = [Buf(), Buf()]
        wus = [S.sb(f"wus{i}", [128, KC * 128], BF16) for i in range(2)]; Bwus = [Buf(), Buf()]
        tmp = [S.sb(f"tmp{i}", [128, 512]) for i in range(2)]; Btmp = [Buf(), Buf()]
        yb = [S.sb(f"yb{i}", [128, D], BF16) for i in range(2)]; Byb = [Buf(), Buf()]
        Bout = Buf()
        xv = xw[:, 0:KC * CAP].rearrange("p (kc t) -> p kc t", t=CAP)
        wdv = xw[:, 0:FC * D].rearrange("p (fc n) -> p fc n", n=D)
        hv = hid[:].rearrange("p (fc t) -> p fc t", t=CAP)
        nt = 0; ny = 0
        for j in range(NJ):
            e = j // JPW
            S.dma(S.sp, xv, XT[j].rearrange("(kc p) t -> p kc t", p=128), writes=[Bxw])
            for fc in range(FC):
                wb = fc % 2
                S.dma(S.pool, wgs[wb][:].rearrange("p (kc f) -> p kc f", f=128), wg[e, :, fc * 128:(fc + 1) * 128].rearrange("(kc p) f -> p kc f", p=128), writes=[Bwgs[wb]])
                S.dma(S.pool, wus[wb][:].rearrange("p (kc f) -> p kc f", f=128), wu[e, :, fc * 128:(fc + 1) * 128].rearrange("(kc p) f -> p kc f", p=128), writes=[Bwus[wb]])
                for tb in range(NTB):
                    for kc in range(KC):
                        S.op(S.pe, ("matmul", dict(out=PS[tb % 4][:, :], lhsT=wgs[wb][:, kc * 128:(kc + 1) * 128], rhs=xv[:, kc, tb * 512:(tb + 1) * 512],
                                                   start=(kc == 0), stop=(kc == KC - 1))), reads=[Bwgs[wb], Bxw], writes=[BPS[tb % 4]])
                    for kc in range(KC):
                        S.op(S.pe, ("matmul", dict(out=PS[4 + tb % 4][:, :], lhsT=wus[wb][:, kc * 128:(kc + 1) * 128], rhs=xv[:, kc, tb * 512:(tb + 1) * 512],
                                                   start=(kc == 0), stop=(kc == KC - 1))), reads=[Bwus[wb], Bxw], writes=[BPS[4 + tb % 4]])
                    ti = nt % 2; nt += 1
                    S.op(S.act, ("activation", dict(out=tmp[ti][:], in_=PS[tb % 4][:, :], func=AF.Silu)), reads=[BPS[tb % 4]], writes=[Btmp[ti]])
                    S.op(S.dve, ("tensor_tensor", dict(out=hv[:, fc, tb * 512:(tb + 1) * 512], in0=tmp[ti][:], in1=PS[4 + tb % 4][:, :], op=ALU.mult)),
                         reads=[Btmp[ti], BPS[4 + tb % 4]], writes=[Bhid])
            for fc in range(FC):
                S.dma(S.pool, wdv[:, fc, :], wd[e, fc * 128:(fc + 1) * 128, :], writes=[Bxw])
            for tt in range(NTT):
                for nb in range(4):
                    for fc in range(FC):
                        S.op(S.pe, ("matmul", dict(out=PS[nb][:, :], lhsT=hv[:, fc, tt * 128:(tt + 1) * 128], rhs=wdv[:, fc, nb * 512:(nb + 1) * 512],
                                                   start=(fc == 0), stop=(fc == FC - 1))), reads=[Bhid, Bxw], writes=[BPS[nb]])
                yi = ny % 2; ny += 1
                gcol = gt[:, j * NTT + tt:j * NTT + tt + 1]
                for nb in range(4):
                    sl = slice(nb * 512, (nb + 1) * 512)
                    if nb % 2 == 0:
                        S.op(S.act, ("activation", dict(out=yb[yi][:, sl], in_=PS[nb][:, :], func=AF.Copy, scale=gcol)), reads=[BPS[nb], Bgt], writes=[Byb[yi]])
                    else:
                        S.op(S.dve, ("tensor_scalar", dict(out=yb[yi][:, sl], in0=PS[nb][:, :], scalar1=gcol, scalar2=None, op0=ALU.mult)), reads=[BPS[nb], Bgt], writes=[Byb[yi]])
                S.dma(S.sp, Y[j, tt * 128:(tt + 1) * 128, :], yb[yi][:], reads=[Byb[yi]], writes=[Bout])
        S.finish([Bout])
        S.emit()
    return nc


import ml_dtypes as _mld
_BF = _mld.bfloat16
_B, _T, _DEPTH = 2, 16384, 2
_TC = 4096
_NEXP, _CAP = 16, 2048
_prog_cache = {}


def _make_consts():
    c = np.zeros((128, 768), np.float32)
    c[:, 0:128] = np.eye(128)
    c[:, 128:256] = 1.0
    s = np.arange(64)[:, None]; t = np.arange(64)[None, :]
    for d in range(2):
        ok = (s <= t) if d == 0 else (s >= t)
        strict = (s < t) if d == 0 else (s > t)
        c[0:64, 256 + d * 64: 320 + d * 64] = ok
        c[0:64, 384 + d * 64: 448 + d * 64] = -1.0 * ok
        c[0:64, 512 + d * 64: 576 + d * 64] = np.where(ok, 0.0, -30000.0)
        c[0:64, 640 + d * 64: 704 + d * 64] = strict
    return c


def _prog(key, fn):
    if key not in _prog_cache:
        _prog_cache[key] = fn()
    return _prog_cache[key]


import time as _time
import sys as _sys
_T0 = [None]


def _log(msg):
    if _T0[0] is None:
        _T0[0] = _time.time()
    print(f"[kernel +{_time.time() - _T0[0]:7.1f}s] {msg}", file=_sys.stderr, flush=True)


def _run(nc, in_maps, tag=""):
    nb = sum(int(np.asarray(v).nbytes) for m in in_maps for v in m.values())
    t0 = _time.time()
    res = run_bass_kernel_spmd(nc, in_maps, core_ids=list(range(len(in_maps))))
    _log(f"launch {tag}: {nb / 1e6:.0f} MB in, {_time.time() - t0:.1f}s")
    return res.results


def _u16(a):
    return np.asarray(a).view(np.uint16)


def _bcast(v):
    return np.ascontiguousarray(np.broadcast_to(np.asarray(v, np.float32)[None, :], (128, v.shape[0])))


def kernel(x, norm_mix, norm_ffn, norm_final, w_in, conv_w, gdn_a_log, gdn_dt_bias, gdn_norm,
           hgrn_lower_bounds, hgrn_norm, w_out, w_router, w_gate, w_up, w_down):
    f32 = np.float32
    x = np.asarray(x, f32)
    norm_mix = np.asarray(norm_mix, f32); norm_ffn = np.asarray(norm_ffn, f32); norm_final = np.asarray(norm_final, f32)
    w_in = np.asarray(w_in, f32); conv_w = np.asarray(conv_w, f32)
    gdn_a_log = np.asarray(gdn_a_log, f32); gdn_dt_bias = np.asarray(gdn_dt_bias, f32)
    gdn_norm = np.asarray(gdn_norm, f32); hgrn_norm = np.asarray(hgrn_norm, f32)
    hgrn_lower_bounds = np.asarray(hgrn_lower_bounds, f32)
    w_out = np.asarray(w_out, f32); w_router = np.asarray(w_router, f32)
    w_gate = np.asarray(w_gate, f32); w_up = np.asarray(w_up, f32); w_down = np.asarray(w_down, f32)
    B, T, TC = _B, _T, _TC
    consts = _make_consts()
    ident = np.eye(128, dtype=f32)
    ones = np.ones((128, 128), f32)
    iota = np.ascontiguousarray(np.broadcast_to(np.arange(128, dtype=f32)[None, :], (128, 128)))
    tok_shard = lambda a, c: np.ascontiguousarray(a[c // 4, (c % 4) * TC:((c % 4) + 1) * TC])

    ncN = _prog(("N",), lambda: build_tok(TC, "N"))
    nwb = _bcast(norm_mix[0])
    _log("start")
    res = _run(ncN, [{"h": tok_shard(x, c), "nwb": nwb} for c in range(8)], "N")
    u = np.stack([np.concatenate([_u16(res[b * 4 + q]["u_out"]) for q in range(4)], axis=0) for b in range(B)])
    h = x
    out = None
    for l in range(_DEPTH):
        ncM, _ = _prog(("M",), lambda: build_mixer(T, 2, 2, depth=_DEPTH))
        uT = [np.ascontiguousarray(u[b].T).view(_BF) for b in range(B)]
        hcm = np.ascontiguousarray(np.broadcast_to((np.arange(_DEPTH) <= l).astype(f32)[None, :], (128, _DEPTH)))
        in_maps = []
        for c in range(8):
            b, g = c // 4, c % 4
            heads = (2 * g, 2 * g + 1)
            fm_cols = []
            for hh in heads:
                fm_cols += list(range(hh * 128, hh * 128 + 128)) + list(range(1024 + hh * 128, 1024 + hh * 128 + 128)) + list(range(2048 + hh * 128, 2048 + hh * 128 + 128))
                fm_cols += [4096 + hh, 4096 + 8 + hh, 4112 + hh, 4112 + 8 + hh]
            for hh in heads:
                fm_cols += list(range(4128 + hh * 128, 4128 + hh * 128 + 128))
                fm_cols += list(range(5152 + hh * 128, 5152 + hh * 128 + 128)) + list(range(5152 + 1024 + hh * 128, 5152 + 1024 + hh * 128 + 128))
            tm_cols = []
            for hh in heads:
                tm_cols += list(range(3072 + hh * 128, 3072 + hh * 128 + 128))
            for hh in heads:
                tm_cols += list(range(7200 + hh * 128, 7200 + hh * 128 + 128)) + list(range(8224 + hh * 128, 8224 + hh * 128 + 128))
            wfm = np.ascontiguousarray(w_in[l][:, fm_cols])
            wtm = np.ascontiguousarray(w_in[l][:, tm_cols])
            gcw = np.zeros((128, 2 * 21), f32)
            gpar = np.zeros((2, 4), f32)
            hlb = np.zeros((128, 2 * 2 * _DEPTH), f32)
            for hi, hh in enumerate(heads):
                for j in range(3):
                    gcw[:, hi * 21 + j * 7: hi * 21 + j * 7 + 7] = conv_w[l][:, j * 1024 + hh * 128: j * 1024 + hh * 128 + 128].T
                gpar[:, hi * 2 + 0] = gdn_dt_bias[l][:, hh]
                gpar[:, hi * 2 + 1] = gdn_a_log[l][:, hh]
                for d in range(2):
                    for dep in range(_DEPTH):
                        hlb[:, (hi * 2 + d) * _DEPTH + dep] = hgrn_lower_bounds[dep, d, hh * 128:(hh + 1) * 128]
            normw = np.ascontiguousarray(np.broadcast_to(np.concatenate([gdn_norm[l], hgrn_norm[l]])[None, :], (128, 256)))
            in_maps.append({"uT": uT[b], "wfm": wfm, "wtm": wtm, "consts": consts, "gcw": gcw, "gpar": gpar, "normw": normw, "hlb": hlb, "hcm": hcm})
        res = _run(ncM, in_maps, f"M{l}")
        mixed = np.zeros((B, T, 2048), np.uint16)
        for c in range(8):
            b, g = c // 4, c % 4
            om = _u16(res[c]["omix"])
            mixed[b, :, (2 * g) * 128:(2 * g + 2) * 128] = om[:, 0:256]
            mixed[b, :, 1024 + (2 * g) * 128:1024 + (2 * g + 2) * 128] = om[:, 256:512]
        del res
        ncO = _prog(("O",), lambda: build_tok(TC, "O"))
        nwb = _bcast(norm_ffn[l])
        in_maps = []
        for c in range(8):
            b, q = c // 4, c % 4
            in_maps.append({"h": tok_shard(h, c), "nwb": nwb, "oT": np.ascontiguousarray(mixed[b, q * TC:(q + 1) * TC].T).view(_BF),
                            "w_out": w_out[l], "w_r": w_router[l], "ident": ident})
        res = _run(ncO, in_maps, f"O{l}")
        u2 = np.stack([np.concatenate([_u16(res[b * 4 + q]["u_out"]) for q in range(4)], axis=0) for b in range(B)])
        probs = np.stack([np.concatenate([res[b * 4 + q]["p_out"] for q in range(4)], axis=0) for b in range(B)])
        del res
        ncK = _prog(("K",), lambda: build_topk(B * _NEXP, 128, _CAP))
        P = np.ascontiguousarray(probs.reshape(B, 128, 128, _NEXP).transpose(1, 0, 3, 2)).reshape(128, B * _NEXP * 128)
        res = _run(ncK, [{"P": P, "ones": ones}], f"K{l}")
        mask = res[0]["mask"].reshape(128, B, _NEXP, 128).transpose(1, 0, 3, 2).reshape(B, T, _NEXP) > 0.5
        idx = np.zeros((B, _NEXP, _CAP), np.int64)
        for b in range(B):
            for e in range(_NEXP):
                sel = np.nonzero(mask[b, :, e])[0]
                if sel.shape[0] != _CAP:
                    pv = probs[b, :, e]
                    order = np.lexsort((np.arange(T), -pv))
                    sel = np.sort(order[:_CAP])
                idx[b, e] = sel
        ncE = _prog(("E",), lambda: build_experts(4, _CAP))
        in_maps = []
        for c in range(8):
            XT = np.zeros((4, 2048, _CAP), np.uint16)
            gates = np.zeros((128, 4 * (_CAP // 128)), f32)
            for j in range(4):
                e = 2 * c + j // 2; b = j % 2
                XT[j] = u2[b][idx[b, e]].T
                gv = probs[b, idx[b, e], e]
                gates[:, j * (_CAP // 128):(j + 1) * (_CAP // 128)] = gv.reshape(_CAP // 128, 128).T
            in_maps.append({"XT": XT.view(_BF), "gates": gates, "wg": np.ascontiguousarray(w_gate[l, 2 * c:2 * c + 2]),
                            "wu": np.ascontiguousarray(w_up[l, 2 * c:2 * c + 2]), "wd": np.ascontiguousarray(w_down[l, 2 * c:2 * c + 2])})
        _log("E inputs ready")
        res = _run(ncE, in_maps, f"E{l}")
        del in_maps
        NTB_ = T // 128
        packs = []
        kch = 1
        for b in range(B):
            Ycat = np.concatenate([_u16(res[e // 2]["Y"])[(e % 2) * 2 + b] for e in range(_NEXP)], axis=0)
            tok = idx[b].reshape(-1)
            order = np.argsort(tok, kind="stable")
            tok_s = tok[order]
            tile = tok_s // 128
            start = np.searchsorted(tile, np.arange(NTB_), side="left")
            rank = np.arange(tok_s.shape[0]) - start[tile]
            kch = max(kch, int((rank.max() // 128) + 1))
            packs.append((Ycat, order, tok_s, tile, rank))
        del res
        last = (l == _DEPTH - 1)
        ncC = _prog(("C", kch, last), lambda: build_tok(TC, "C", KCH=kch, last=last))
        nwb = _bcast(norm_final if last else norm_mix[l + 1])
        in_maps = []
        for b in range(B):
            Ycat, order, tok_s, tile, rank = packs[b]
            rows = np.zeros((NTB_, kch, 128, 2048), np.uint16)
            rows[tile, rank // 128, rank % 128] = Ycat[order]
            ixa = np.full((NTB_, kch, 128), -1.0, f32)
            ixa[tile, rank // 128, rank % 128] = (tok_s % 128).astype(f32)
            for q in range(4):
                tl = slice(q * 32, (q + 1) * 32)
                in_maps.append({"h": np.ascontiguousarray(h[b, q * TC:(q + 1) * TC]), "nwb": nwb,
                                "oT": np.ascontiguousarray(mixed[b, q * TC:(q + 1) * TC].T).view(_BF), "w_out": w_out[l],
                                "rows": np.ascontiguousarray(rows[tl]).view(_BF),
                                "idx": np.ascontiguousarray(ixa[tl].transpose(2, 0, 1)).reshape(128, 32 * kch),
                                "iota": iota})
            del rows
        del packs
        _log(f"C inputs ready kch={kch}")
        res = _run(ncC, in_maps, f"C{l}")
        del in_maps
        if last:
            out = np.stack([np.concatenate([res[b * 4 + q]["f_out"] for q in range(4)], axis=0) for b in range(B)]).astype(f32)
        else:
            h = np.stack([np.concatenate([res[b * 4 + q]["h_out"] for q in range(4)], axis=0) for b in range(B)])
            u = np.stack([np.concatenate([_u16(res[b * 4 + q]["u_out"]) for q in range(4)], axis=0) for b in range(B)])
        del res
    return out
```

```python
import contextlib
import numpy as np
import concourse.bass as bass
import concourse.mybir as mybir
from concourse.bass_utils import run_bass_kernel_spmd

F32 = mybir.dt.float32
BF16 = mybir.dt.bfloat16
I32 = mybir.dt.int32
AF = mybir.ActivationFunctionType
ALU = mybir.AluOpType
AX = mybir.AxisListType


class Buf:
    __slots__ = ("name", "last_w", "readers", "excl")

    def __init__(self, name="", excl=False):
        self.name = name
        self.last_w = None
        self.readers = {}
        self.excl = excl


class Eng:
    def __init__(self, name, handle, sem, is_pe=False):
        self.name = name
        self.h = handle
        self.sem = sem
        self.count = 0
        self.waited = {}
        self.is_pe = is_pe
        self.dma_sems = []
        self.dma_tot = []
        self.dma_rr = 0


class Sched:
    def __init__(self, nc, stack, n_dma_sems=10):
        self.nc = nc
        self.stack = stack
        mk = lambda n: stack.enter_context(nc.semaphore(n))
        self.pe = Eng("pe", nc.tensor, mk("s_pe"), is_pe=True)
        self.act = Eng("act", nc.scalar, mk("s_act"))
        self.dve = Eng("dve", nc.vector, mk("s_dve"))
        self.pool = Eng("pool", nc.gpsimd, mk("s_pool"))
        self.sp = Eng("sp", nc.sync, mk("s_sp"))
        for e in (self.sp, self.pool, self.act):
            for j in range(n_dma_sems):
                e.dma_sems.append(mk(f"d_{e.name}{j}"))
                e.dma_tot.append(0)
        self.semkey = {}
        self.n_ops = 0
        self.prog = {e.name: [] for e in (self.pe, self.act, self.dve, self.pool, self.sp)}
        self.cur_waits = None

    def sb(self, name, shape, dt=F32, stack=None):
        t = (stack or self.stack).enter_context(self.nc.sbuf_tensor(name, list(shape), dt))
        return t

    def barrier(self):
        engs = (self.pe, self.act, self.dve, self.pool, self.sp)
        toks = []
        for e in engs:
            if e.count:
                toks.append((e.sem, e.count))
            for sem, tot in zip(e.dma_sems, e.dma_tot):
                if tot:
                    toks.append((sem, tot))
        for e in engs:
            for sem, val in toks:
                self._wait(e, sem, val)

    def ps(self, name, shape, dt=F32):
        t = self.stack.enter_context(self.nc.psum_tensor(name, list(shape), dt))
        return t

    def _wait(self, eng, sem, val):
        k = id(sem)
        if eng.waited.get(k, 0) >= val:
            return
        self.prog[eng.name].append(("wait", sem, val))
        eng.waited[k] = val

    def op(self, eng, fn, reads=(), writes=(), dma=False):
        ex = [b for b in reads if b.excl]
        if ex:
            reads = [b for b in reads if not b.excl]
            writes = list(writes) + [b for b in ex if b not in writes]
        deps = {}

        def add(tok):
            if tok is None:
                return
            sem, val = tok
            k = id(sem)
            if k not in deps or deps[k][1] < val:
                deps[k] = (sem, val)

        for b in reads:
            add(b.last_w)
        for b in writes:
            add(b.last_w)
            for tok in b.readers.values():
                add(tok)
        for k, (sem, val) in deps.items():
            if eng.is_pe and sem is eng.sem:
                continue
            self._wait(eng, sem, val)
        if dma:
            j = eng.dma_rr
            eng.dma_rr = (j + 1) % len(eng.dma_sems)
            sem = eng.dma_sems[j]
            if eng.dma_tot[j] > 0:
                self._wait(eng, sem, eng.dma_tot[j])
            self.prog[eng.name].append(("op", fn, sem, 16))
            eng.dma_tot[j] += 16
            tok = (sem, eng.dma_tot[j])
        else:
            self.prog[eng.name].append(("op", fn, eng.sem, 1))
            eng.count += 1
            tok = (eng.sem, eng.count)
        for b in reads:
            b.readers[id(tok[0])] = tok
        for b in writes:
            b.last_w = tok
            b.readers = {}
        self.n_ops += 1
        return tok

    def dma(self, eng, out, in_, reads=(), writes=(), **kw):
        if eng is self.sp:
            self._dq = getattr(self, "_dq", 0) + 1
            if self._dq % 2 == 0:
                eng = self.pool
        return self.op(eng, ("dma_start", dict(out=out, in_=in_, **kw)), reads, writes, dma=True)

    def emit(self):
        nc = self.nc
        def run(h, items):
            for it in items:
                if it[0] == "wait":
                    h.wait_ge(it[1], it[2])
                else:
                    name, kw = it[1]
                    ins = getattr(h, name)(**kw)
                    ins.then_inc(it[2], it[3])
        with nc.Block() as block:
            @block.tensor
            def _(h):
                run(h, self.prog["pe"])
            @block.scalar
            def _(h):
                run(h, self.prog["act"])
            @block.vector
            def _(h):
                run(h, self.prog["dve"])
            @block.gpsimd
            def _(h):
                run(h, self.prog["pool"])
            @block.sync
            def _(h):
                run(h, self.prog["sp"])

    def finish(self, bufs):
        for b in bufs:
            if b.last_w is not None:
                self._wait(self.sp, b.last_w[0], b.last_w[1])


D = 2048
KC = D // 128
HD = 128
C = 64


def build_mixer(T, n_g, n_h, debug_outputs=(), layer=0, depth=2):
    nc = bass.Bass("TRN2", target_bir_lowering=False)
    NB = T // 512
    fm_tiles = []
    for h in range(n_g):
        fm_tiles += [(f"gq{h}", 128), (f"gk{h}", 128), (f"gv{h}", 128), (f"gb{h}", 2), (f"ga{h}", 2)]
    for h in range(n_h):
        fm_tiles += [(f"hq{h}", 128), (f"hf0{h}", 128), (f"hf1{h}", 128)]
    NFM = sum(n for _, n in fm_tiles)
    NTM = n_g * 128 + n_h * 256
    uT = nc.dram_tensor("uT", [D, T], BF16, kind="ExternalInput").ap()
    wfm = nc.dram_tensor("wfm", [D, NFM], F32, kind="ExternalInput").ap()
    wtm = nc.dram_tensor("wtm", [D, NTM], F32, kind="ExternalInput").ap()
    NCST = 768
    consts = nc.dram_tensor("consts", [128, NCST], F32, kind="ExternalInput").ap()
    if n_g:
        gcw = nc.dram_tensor("gcw", [128, n_g * 21], F32, kind="ExternalInput").ap()
        gpar = nc.dram_tensor("gpar", [2, n_g * 2], F32, kind="ExternalInput").ap()
    normw = nc.dram_tensor("normw", [128, 256], F32, kind="ExternalInput").ap()
    DEPTH_ = depth; LAYER_ = layer
    if n_h:
        hlb = nc.dram_tensor("hlb", [128, n_h * 2 * depth], F32, kind="ExternalInput").ap()
        hcm = nc.dram_tensor("hcm", [128, depth], F32, kind="ExternalInput").ap()
    dbg = {}
    ALLOUT = []
    def scratch(name, shape, dt=F32):
        kind = "ExternalOutput" if name in debug_outputs else "Internal"
        t = nc.dram_tensor(name, list(shape), dt, kind=kind).ap()
        dbg[name] = t
        return t
    FMs = {}
    for name, n in fm_tiles:
        FMs[name] = scratch("fm_" + name, [n, T])
    TMs = scratch("tm", [T, NTM])
    B_FM = {name: Buf() for name, _ in fm_tiles}
    B_TM = Buf()

    with contextlib.ExitStack() as st:
        S = Sched(nc, st)
        PS = [S.ps(f"ps{i}", [128, 512]) for i in range(8)]
        BPS = [Buf(f"ps{i}", excl=True) for i in range(8)]
        phA = contextlib.ExitStack()
        wfm_sb = S.sb("wfm_sb", [128, KC, NFM], BF16, phA); Bwfm = Buf()
        wtm_sb = S.sb("wtm_sb", [128, KC, NTM], BF16, phA); Bwtm = Buf()
        for kc in range(KC):
            S.dma(S.pool, wfm_sb[:, kc, :], wfm[kc * 128:(kc + 1) * 128, :], writes=[Bwfm])
            S.dma(S.pool, wtm_sb[:, kc, :], wtm[kc * 128:(kc + 1) * 128, :], writes=[Bwtm])
        ut = [S.sb(f"ut{i}", [128, KC, 512], BF16, phA) for i in range(2)]; But = [Buf() for _ in range(2)]
        stg = [S.sb(f"stg{i}", [128, 512], F32, phA) for i in range(4)]; Bstg = [Buf() for _ in range(4)]
        uTv = uT.rearrange("(kc p) t -> p kc t", p=128)
        nstg = 0
        npb = 0
        for blk in range(NB):
            ub = blk % 2
            S.dma(S.sp, ut[ub][:], uTv[:, :, blk * 512:(blk + 1) * 512], writes=[But[ub]])
            col = 0
            for name, n in fm_tiles:
                pb = npb % 4; npb += 1
                for kc in range(KC):
                    S.op(S.pe, ("matmul", dict(out=PS[pb][0:n, :], lhsT=wfm_sb[:, kc, col:col + n], rhs=ut[ub][:, kc, :],
                                               start=(kc == 0), stop=(kc == KC - 1))),
                         reads=[Bwfm, But[ub]], writes=[BPS[pb]])
                sg = nstg % 4; nstg += 1
                eng = S.act if sg % 2 == 0 else S.dve
                if eng is S.act:
                    S.op(S.act, ("copy", dict(out=stg[sg][0:n, :], in_=PS[pb][0:n, :])), reads=[BPS[pb]], writes=[Bstg[sg]])
                else:
                    S.op(S.dve, ("tensor_copy", dict(out=stg[sg][0:n, :], in_=PS[pb][0:n, :])), reads=[BPS[pb]], writes=[Bstg[sg]])
                S.dma(S.sp, FMs[name][:, blk * 512:(blk + 1) * 512], stg[sg][0:n, :], reads=[Bstg[sg]], writes=[B_FM[name]])
                col += n
            for tt in range(4):
                t0 = blk * 512 + tt * 128
                for c0 in range(0, NTM, 512):
                    cn = min(512, NTM - c0)
                    pb = npb % 4; npb += 1
                    for kc in range(KC):
                        S.op(S.pe, ("matmul", dict(out=PS[pb][:, 0:cn], lhsT=ut[ub][:, kc, tt * 128:(tt + 1) * 128], rhs=wtm_sb[:, kc, c0:c0 + cn],
                                                   start=(kc == 0), stop=(kc == KC - 1))),
                             reads=[Bwtm, But[ub]], writes=[BPS[pb]])
                    sg = nstg % 4; nstg += 1
                    if sg % 2 == 0:
                        S.op(S.act, ("copy", dict(out=stg[sg][:, 0:cn], in_=PS[pb][:, 0:cn])), reads=[BPS[pb]], writes=[Bstg[sg]])
                    else:
                        S.op(S.dve, ("tensor_copy", dict(out=stg[sg][:, 0:cn], in_=PS[pb][:, 0:cn])), reads=[BPS[pb]], writes=[Bstg[sg]])
                    S.dma(S.sp, TMs[t0:t0 + 128, c0:c0 + cn], stg[sg][:, 0:cn], reads=[Bstg[sg]], writes=[B_TM])
        S.barrier()
        phA.close()
        cst = S.sb("cst", [128, NCST]); Bc = Buf()
        S.dma(S.sp, cst[:], consts[:, :], writes=[Bc])
        ident = cst[:, 0:128]
        ones = cst[:, 128:256]
        def TRI(d): return cst[0:64, 256 + d * 64: 256 + d * 64 + 64]
        def NTRI(d): return cst[0:64, 384 + d * 64: 384 + d * 64 + 64]
        def NEGM(d): return cst[0:64, 512 + d * 64: 512 + d * 64 + 64]
        def SMASK(d): return cst[0:64, 640 + d * 64: 640 + d * 64 + 64]
        I64 = cst[0:64, 0:64]
        def bc8(ap64):
            return ap64.unsqueeze(1).broadcast_to([64, 8, 64])
        def v8(t):
            return t.rearrange("p (a b) -> p a b", b=64)
        NCH = T // C
        NG = NCH // 8
        pbc = [0]
        def nextpb(lo=0, hi=8):
            pbc[0] += 1
            return lo + pbc[0] % (hi - lo)

        gsm = {}
        for h in range(n_g):
            for d in range(2):
                gsm[(h, d)] = dict(
                    b=S.sb(f"g_b{h}{d}", [64, NCH]), neg=S.sb(f"g_neg{h}{d}", [64, NCH]),
                    bkd=S.sb(f"g_bkd{h}{d}", [64, NCH]), gl=S.sb(f"g_gl{h}{d}", [128, NCH]), B=Buf())
        hsm = {}
        for h in range(n_h):
            for d in range(2):
                hsm[(h, d)] = dict(el=S.sb(f"h_el{h}{d}", [128, NCH]), B=Buf())

        TB = min(T, 2048)
        NTB = T // TB
        KhT = {}; QhT = {}; Ktok = {}; Vtok = {}; GB = {}
        BKhT = {}; BQhT = {}; BKtok = {}; BVtok = {}; BGB = {}
        for h in range(n_g):
            KhT[h] = scratch(f"KhT{h}", [128, T], BF16); QhT[h] = scratch(f"QhT{h}", [128, T], BF16)
            Ktok[h] = scratch(f"Ktok{h}", [T, 128], BF16); Vtok[h] = scratch(f"Vtok{h}", [T, 128], F32)
            GB[h] = scratch(f"GB{h}", [4, T], F32)
            BKhT[h] = Buf(); BQhT[h] = Buf(); BKtok[h] = Buf(); BVtok[h] = Buf(); BGB[h] = Buf()
            ALLOUT += [BKhT[h], BQhT[h], BKtok[h], BVtok[h], BGB[h]]
        if n_g:
          with contextlib.ExitStack() as ph:
            cw = S.sb("cw", [128, n_g * 21], F32, ph); Bcw = Buf()
            S.dma(S.sp, cw[:], gcw[:, :], writes=[Bcw])
            gp = S.sb("gp", [2, n_g * 2], F32, ph); Bgp = Buf()
            S.dma(S.sp, gp[:], gpar[:, :], writes=[Bgp])
            xin = [S.sb(f"xin{i}", [128, TB + 6], F32, ph) for i in range(2)]; Bxin = [Buf(), Buf()]
            acc = S.sb("acc", [128, TB], F32, ph); Bacc = Buf()
            yy = S.sb("yy", [128, TB], F32, ph); Byy = Buf()
            sq = S.sb("sq", [128, TB], F32, ph); Bsq = Buf()
            kh = S.sb("kh", [128, TB], F32, ph); Bkh = Buf()
            obf = [S.sb(f"obf{i}", [128, TB], BF16, ph) for i in range(2)]; Bobf = [Buf(), Buf()]
            rs = S.sb("rs", [128, 512], F32, ph); Brs = Buf()
            tst = [S.sb(f"tst{i}", [128, 512], F32, ph) for i in range(2)]; Btst = [Buf(), Buf()]
            tsb = [S.sb(f"tsb{i}", [128, 512], BF16, ph) for i in range(2)]; Btsb = [Buf(), Buf()]
            BT = min(T, 4096)
            bt = S.sb("bt", [2, BT], F32, ph); Bbt = Buf()
            at = S.sb("at", [2, BT], F32, ph); Bat = Buf()
            na = S.sb("na", [2, 2], F32, ph); Bna = Buf()
            nx = 0; nob = 0; nts = 0
            for h in range(n_g):
                for j, nm in enumerate(("gq", "gk", "gv")):
                    src = FMs[f"{nm}{h}"]
                    for tb in range(NTB):
                        t0 = tb * TB
                        xi = nx % 2; nx += 1
                        lo = max(t0 - 3, 0); hi = min(t0 + TB + 3, T)
                        if t0 == 0:
                            S.op(S.pool, ("memset", dict(ap=xin[xi][:, 0:3], constant=0.0)), writes=[Bxin[xi]])
                        if t0 + TB == T:
                            S.op(S.pool, ("memset", dict(ap=xin[xi][:, TB + 3:TB + 6], constant=0.0)), writes=[Bxin[xi]])
                        S.dma(S.sp, xin[xi][:, lo - (t0 - 3): hi - (t0 - 3)], src[:, lo:hi], reads=[B_FM[f"{nm}{h}"]], writes=[Bxin[xi]])
                        cb = h * 21 + j * 7
                        S.op(S.dve, ("tensor_scalar", dict(out=acc[:], in0=xin[xi][:, 0:TB], scalar1=cw[:, cb:cb + 1], scalar2=None, op0=ALU.mult)),
                             reads=[Bxin[xi], Bcw], writes=[Bacc])
                        for tap in range(1, 7):
                            S.op(S.dve, ("scalar_tensor_tensor", dict(out=acc[:], in0=xin[xi][:, tap:tap + TB], scalar=cw[:, cb + tap:cb + tap + 1], in1=acc[:], op0=ALU.mult, op1=ALU.add)),
                                 reads=[Bxin[xi], Bcw, Bacc], writes=[Bacc])
                        S.op(S.act, ("activation", dict(out=yy[:], in_=acc[:], func=AF.Silu)), reads=[Bacc], writes=[Byy])
                        if nm == "gv":
                            tsrc, Btsrc = yy, Byy
                        else:
                            S.op(S.act, ("activation", dict(out=sq[:], in_=yy[:], func=AF.Square)), reads=[Byy], writes=[Bsq])
                            ob = nob % 2; nob += 1
                            for sbk in range(TB // 512):
                                sl = slice(sbk * 512, (sbk + 1) * 512)
                                pb = nextpb()
                                S.op(S.pe, ("matmul", dict(out=PS[pb][:, :], lhsT=ones, rhs=sq[:, sl], start=True, stop=True)), reads=[Bc, Bsq], writes=[BPS[pb]])
                                S.op(S.dve, ("tensor_scalar", dict(out=rs[:], in0=PS[pb][:, :], scalar1=1e-6, scalar2=None, op0=ALU.add)), reads=[BPS[pb]], writes=[Brs])
                                S.op(S.act, ("sqrt", dict(out=rs[:], in_=rs[:])), reads=[Brs], writes=[Brs])
                                S.op(S.dve, ("reciprocal", dict(out=rs[:], in_=rs[:])), reads=[Brs], writes=[Brs])
                                if nm == "gk":
                                    S.op(S.dve, ("tensor_tensor", dict(out=kh[:, sl], in0=yy[:, sl], in1=rs[:], op=ALU.mult)), reads=[Byy, Brs], writes=[Bkh])
                                else:
                                    S.op(S.dve, ("scalar_tensor_tensor", dict(out=obf[ob][:, sl], in0=yy[:, sl], scalar=float(HD) ** -0.5, in1=rs[:], op0=ALU.mult, op1=ALU.mult)),
                                         reads=[Byy, Brs], writes=[Bobf[ob]])
                            if nm == "gk":
                                S.op(S.act, ("copy", dict(out=obf[ob][:], in_=kh[:])), reads=[Bkh], writes=[Bobf[ob]])
                                S.dma(S.sp, KhT[h][:, t0:t0 + TB], obf[ob][:], reads=[Bobf[ob]], writes=[BKhT[h]])
                                tsrc, Btsrc = kh, Bkh
                            else:
                                S.dma(S.sp, QhT[h][:, t0:t0 + TB], obf[ob][:], reads=[Bobf[ob]], writes=[BQhT[h]])
                                tsrc = None
                        if tsrc is not None:
                            for sbk in range(TB // 512):
                                pb = nextpb()
                                for jj in range(4):
                                    c0 = sbk * 512 + jj * 128
                                    S.op(S.pe, ("transpose", dict(out=PS[pb][:, jj * 128:(jj + 1) * 128], in_=tsrc[:, c0:c0 + 128], identity=ident)),
                                         reads=[Bc, Btsrc], writes=[BPS[pb]])
                                ts = nts % 2; nts += 1
                                tt0 = t0 + sbk * 512
                                if nm == "gv":
                                    S.op(S.act, ("copy", dict(out=tst[ts][:], in_=PS[pb][:, :])), reads=[BPS[pb]], writes=[Btst[ts]])
                                    S.dma(S.sp, Vtok[h][tt0:tt0 + 512, :].rearrange("(j p) d -> p j d", p=128), tst[ts][:].rearrange("p (j d) -> p j d", d=128),
                                          reads=[Btst[ts]], writes=[BVtok[h]])
                                else:
                                    S.op(S.act, ("copy", dict(out=tsb[ts][:], in_=PS[pb][:, :])), reads=[BPS[pb]], writes=[Btsb[ts]])
                                    S.dma(S.sp, Ktok[h][tt0:tt0 + 512, :].rearrange("(j p) d -> p j d", p=128), tsb[ts][:].rearrange("p (j d) -> p j d", d=128),
                                          reads=[Btsb[ts]], writes=[BKtok[h]])
                S.op(S.act, ("activation", dict(out=na[:, 0:1], in_=gp[:, h * 2 + 1:h * 2 + 2], func=AF.Exp)), reads=[Bgp], writes=[Bna])
                S.op(S.dve, ("tensor_scalar", dict(out=na[:, 1:2], in0=na[:, 0:1], scalar1=-1.0, scalar2=None, op0=ALU.mult)), reads=[Bna], writes=[Bna])
                for t0 in range(0, T, BT):
                    S.dma(S.sp, bt[:], FMs[f"gb{h}"][:, t0:t0 + BT], reads=[B_FM[f"gb{h}"]], writes=[Bbt])
                    S.op(S.act, ("activation", dict(out=bt[:], in_=bt[:], func=AF.Sigmoid)), reads=[Bbt], writes=[Bbt])
                    S.dma(S.sp, GB[h][0:2, t0:t0 + BT], bt[:], reads=[Bbt], writes=[BGB[h]])
                    S.dma(S.sp, at[:], FMs[f"ga{h}"][:, t0:t0 + BT], reads=[B_FM[f"ga{h}"]], writes=[Bat])
                    S.op(S.act, ("activation", dict(out=at[:], in_=at[:], func=AF.Exp, bias=gp[:, h * 2:h * 2 + 1])), reads=[Bat, Bgp], writes=[Bat])
                    S.op(S.act, ("activation", dict(out=at[:], in_=at[:], func=AF.Ln, bias=1.0)), reads=[Bat], writes=[Bat])
                    S.op(S.dve, ("tensor_scalar", dict(out=at[:], in0=at[:], scalar1=na[:, 1:2], scalar2=None, op0=ALU.mult)), reads=[Bat, Bna], writes=[Bat])
                    S.dma(S.sp, GB[h][2:4, t0:t0 + BT], at[:], reads=[Bat], writes=[BGB[h]])
          S.barrier()
        QG = {}; ZT = {}; QKM = {}; OG = {}
        BQG = {}; BZT = {}; BQKM = {}; BOG = {}
        for h in range(n_g):
            for d in range(2):
                QG[(h, d)] = scratch(f"QG{h}{d}", [128, T], BF16)
                ZT[(h, d)] = scratch(f"ZT{h}{d}", [NG, 64, 512], BF16)
                QKM[(h, d)] = scratch(f"QKM{h}{d}", [NG, 64, 512], BF16)
                OG[(h, d)] = scratch(f"OG{h}{d}", [T, 128], F32)
                BQG[(h, d)] = Buf(); BZT[(h, d)] = Buf(); BQKM[(h, d)] = Buf(); BOG[(h, d)] = Buf()
                ALLOUT += [BQG[(h, d)], BZT[(h, d)], BQKM[(h, d)]]
        if n_g:
          with contextlib.ExitStack() as ph:
            def gdn_prep(h, d, tg, blo, bhi):
                cnt = [0]
                def npb():
                    cnt[0] += 1
                    return blo + cnt[0] % (bhi - blo)
                g_sn = S.sb(f"g_sn{tg}", [64, NCH], F32, ph); Bg = Buf()
                gc_sb = S.sb(f"gc_sb{tg}", [64, NCH], F32, ph); Bgc = Buf()
                tmpT = [S.sb(f"tmpT{tg}{i}", [128, 64], F32, ph) for i in range(2)]; BtmpT = [Buf(), Buf()]
                kg_ = [S.sb(f"kg{tg}{i}", [128, 512], BF16, ph) for i in range(2)]; Bkg = [Buf(), Buf()]
                qg_ = [S.sb(f"qgi{tg}{i}", [128, 512], BF16, ph) for i in range(2)]; Bqg = [Buf(), Buf()]
                gb8 = S.sb(f"gb8{tg}", [64, 8 * 128], F32, ph); Bgb8 = Buf()
                egcb = S.sb(f"egcb{tg}", [128, 512], F32, ph); Begcb = Buf()
                qgo = [S.sb(f"qgo{tg}{i}", [128, 512], BF16, ph) for i in range(2)]; Bqgo = [Buf(), Buf()]
                Dm = S.sb(f"Dm{tg}", [64, 512], F32, ph); BDm = Buf()
                t1 = S.sb(f"t1{tg}", [64, 512], F32, ph); Bt1 = Buf()
                qkm = [S.sb(f"qkm{tg}{i}", [64, 512], BF16, ph) for i in range(2)]; Bqkm = [Buf(), Buf()]
                A_ = [S.sb(f"A{tg}{i}", [64, 512], F32, ph) for i in range(2)]; BA = [Buf(), Buf()]
                AT_ = [S.sb(f"AT{tg}{i}", [64, 512], F32, ph) for i in range(2)]; BAT = [Buf(), Buf()]
                Pm = S.sb(f"Pm{tg}", [64, 512], F32, ph); BP = Buf()
                zto = [S.sb(f"zto{tg}{i}", [64, 512], BF16, ph) for i in range(2)]; Bzto = [Buf(), Buf()]
                ntm_ = 0; ngrp = 0
                sm = gsm[(h, d)]
                for r, dst, Bdst in ((d, sm["b"], sm["B"]), (2 + d, g_sn, Bg)):
                    for n0 in range(0, NCH, 128):
                        nn = min(128, NCH - n0)
                        ti = ntm_ % 2; ntm_ += 1
                        S.dma(S.sp, tmpT[ti][0:nn, :], GB[h][r, n0 * 64:(n0 + nn) * 64].rearrange("(n s) -> n s", s=64), reads=[BGB[h]], writes=[BtmpT[ti]])
                        pb = npb()
                        S.op(S.pe, ("transpose", dict(out=PS[pb][0:64, 0:nn], in_=tmpT[ti][0:nn, :], identity=cst[0:nn, 0:nn])), reads=[Bc, BtmpT[ti]], writes=[BPS[pb]])
                        S.op(S.act, ("copy", dict(out=dst[:, n0:n0 + nn], in_=PS[pb][0:64, 0:nn])), reads=[BPS[pb]], writes=[Bdst])
                        yield
                pa = npb(); pbk = npb()
                S.op(S.pe, ("matmul", dict(out=PS[pa][0:64, 0:NCH], lhsT=TRI(d), rhs=g_sn[:], start=True, stop=True)), reads=[Bc, Bg], writes=[BPS[pa]])
                S.op(S.act, ("copy", dict(out=gc_sb[:], in_=PS[pa][0:64, 0:NCH])), reads=[BPS[pa]], writes=[Bgc])
                S.op(S.pe, ("matmul", dict(out=PS[pbk][:, 0:NCH], lhsT=cst[0:64, 128:256], rhs=g_sn[:], start=True, stop=True)), reads=[Bc, Bg], writes=[BPS[pbk]])
                S.op(S.act, ("activation", dict(out=sm["gl"][:], in_=PS[pbk][:, 0:NCH], func=AF.Exp)), reads=[BPS[pbk]], writes=[sm["B"]])
                S.op(S.act, ("activation", dict(out=sm["neg"][:], in_=gc_sb[:], func=AF.Exp)), reads=[Bgc], writes=[sm["B"]])
                S.op(S.dve, ("tensor_scalar", dict(out=sm["neg"][:], in0=sm["neg"][:], scalar1=-1.0, scalar2=None, op0=ALU.mult)), reads=[sm["B"]], writes=[sm["B"]])
                S.op(S.dve, ("tensor_tensor", dict(out=sm["bkd"][:], in0=PS[pbk][0:64, 0:NCH], in1=gc_sb[:], op=ALU.subtract)), reads=[BPS[pbk], Bgc], writes=[sm["B"]])
                S.op(S.act, ("activation", dict(out=sm["bkd"][:], in_=sm["bkd"][:], func=AF.Exp)), reads=[sm["B"]], writes=[sm["B"]])
                S.op(S.dve, ("tensor_tensor", dict(out=sm["bkd"][:], in0=sm["bkd"][:], in1=sm["b"][:], op=ALU.mult)), reads=[sm["B"]], writes=[sm["B"]])
                yield
                for cg in range(NG):
                    gi = ngrp % 2; ngrp += 1
                    c0 = cg * 8
                    tk = slice(cg * 512, (cg + 1) * 512)
                    S.dma(S.sp, kg_[gi][:], KhT[h][:, tk], reads=[BKhT[h]], writes=[Bkg[gi]])
                    S.dma(S.sp, qg_[gi][:], QhT[h][:, tk], reads=[BQhT[h]], writes=[Bqg[gi]])
                    gb8v = gb8[:].rearrange("p (a b) -> p a b", b=128)
                    S.op(S.dve, ("tensor_copy", dict(out=gb8v, in_=g_sn[:, c0:c0 + 8].unsqueeze(2).broadcast_to([64, 8, 128]))), reads=[Bg], writes=[Bgb8])
                    yield
                    p1 = npb()
                    for i in range(8):
                        S.op(S.pe, ("matmul", dict(out=PS[p1][:, i * 64:(i + 1) * 64], lhsT=gb8[:, i * 128:(i + 1) * 128], rhs=TRI(d), start=True, stop=True)),
                             reads=[Bgb8, Bc], writes=[BPS[p1]])
                    S.op(S.act, ("activation", dict(out=egcb[:], in_=PS[p1][:, :], func=AF.Exp)), reads=[BPS[p1]], writes=[Begcb])
                    p2 = npb()
                    for i in range(8):
                        o_ = PS[p2][0:64, i * 64:(i + 1) * 64]
                        gbi = gb8[:, i * 128:i * 128 + 64]
                        S.op(S.pe, ("matmul", dict(out=o_, lhsT=gbi, rhs=TRI(d), start=True, stop=False)), reads=[Bgb8, Bc], writes=[BPS[p2]])
                        S.op(S.pe, ("matmul", dict(out=o_, lhsT=NTRI(d), rhs=gbi, start=False, stop=False)), reads=[Bgb8, Bc], writes=[BPS[p2]])
                        S.op(S.pe, ("matmul", dict(out=o_, lhsT=I64, rhs=NEGM(d), start=False, stop=True)), reads=[Bc], writes=[BPS[p2]])
                    S.op(S.act, ("activation", dict(out=Dm[:], in_=PS[p2][0:64, :], func=AF.Exp)), reads=[BPS[p2]], writes=[BDm])
                    yield
                    S.op(S.dve, ("tensor_tensor", dict(out=qgo[gi][:], in0=qg_[gi][:], in1=egcb[:], op=ALU.mult)), reads=[Bqg[gi], Begcb], writes=[Bqgo[gi]])
                    S.dma(S.sp, QG[(h, d)][:, tk], qgo[gi][:], reads=[Bqgo[gi]], writes=[BQG[(h, d)]])
                    p3 = npb()
                    for i in range(8):
                        ck = slice(i * 64, (i + 1) * 64)
                        S.op(S.pe, ("matmul", dict(out=PS[p3][0:64, ck], lhsT=kg_[gi][:, ck], rhs=kg_[gi][:, ck], start=True, stop=True)), reads=[Bkg[gi]], writes=[BPS[p3]])
                    S.op(S.dve, ("tensor_tensor", dict(out=t1[:], in0=PS[p3][0:64, :], in1=Dm[:], op=ALU.mult)), reads=[BPS[p3], BDm], writes=[Bt1])
                    p4 = npb()
                    for i in range(8):
                        ck = slice(i * 64, (i + 1) * 64)
                        S.op(S.pe, ("matmul", dict(out=PS[p4][0:64, ck], lhsT=kg_[gi][:, ck], rhs=qg_[gi][:, ck], start=True, stop=True)), reads=[Bkg[gi], Bqg[gi]], writes=[BPS[p4]])
                    S.op(S.dve, ("tensor_tensor", dict(out=qkm[gi][:], in0=PS[p4][0:64, :], in1=Dm[:], op=ALU.mult)), reads=[BPS[p4], BDm], writes=[Bqkm[gi]])
                    S.dma(S.sp, QKM[(h, d)][cg], qkm[gi][:], reads=[Bqkm[gi]], writes=[BQKM[(h, d)]])
                    yield
                    S.op(S.pool, ("tensor_tensor", dict(out=v8(t1[:]), in0=v8(t1[:]), in1=sm["b"][:, c0:c0 + 8].unsqueeze(2).broadcast_to([64, 8, 64]), op=ALU.mult)),
                         reads=[Bt1, sm["B"]], writes=[Bt1])
                    A = A_[0]; AT = AT_[0]; BAc = BA[0]; BATc = BAT[0]
                    S.op(S.pool, ("tensor_tensor", dict(out=v8(A[:]), in0=v8(t1[:]), in1=bc8(SMASK(d)), op=ALU.mult)), reads=[Bt1, Bc], writes=[BAc])
                    yield
                    p5 = npb()
                    for i in range(8):
                        ck = slice(i * 64, (i + 1) * 64)
                        S.op(S.pe, ("transpose", dict(out=PS[p5][0:64, ck], in_=A[:, ck], identity=I64)), reads=[BAc, Bc], writes=[BPS[p5]])
                    S.op(S.act, ("copy", dict(out=AT[:], in_=PS[p5][0:64, :])), reads=[BPS[p5]], writes=[BATc])
                    S.op(S.dve, ("scalar_tensor_tensor", dict(out=v8(Pm[:]), in0=v8(A[:]), scalar=-1.0, in1=bc8(I64), op0=ALU.mult, op1=ALU.add)), reads=[BAc, Bc], writes=[BP])
                    yield
                    cur = 0
                    for lvl in range(5):
                        nxt = 1 - cur
                        A, AT, A2, A2T = A_[cur], AT_[cur], A_[nxt], AT_[nxt]
                        px = npb()
                        for i in range(8):
                            ck = slice(i * 64, (i + 1) * 64)
                            S.op(S.pe, ("matmul", dict(out=PS[px][0:64, ck], lhsT=A[:, ck], rhs=AT[:, ck], start=True, stop=True)), reads=[BA[cur], BAT[cur]], writes=[BPS[px]])
                        S.op(S.act, ("copy", dict(out=A2T[:], in_=PS[px][0:64, :])), reads=[BPS[px]], writes=[BAT[nxt]])
                        if lvl < 4:
                            py = npb()
                            for i in range(8):
                                ck = slice(i * 64, (i + 1) * 64)
                                S.op(S.pe, ("matmul", dict(out=PS[py][0:64, ck], lhsT=AT[:, ck], rhs=A[:, ck], start=True, stop=True)), reads=[BA[cur], BAT[cur]], writes=[BPS[py]])
                            S.op(S.dve, ("tensor_copy", dict(out=A2[:], in_=PS[py][0:64, :])), reads=[BPS[py]], writes=[BA[nxt]])
                        yield
                        pz = npb()
                        for i in range(8):
                            ck = slice(i * 64, (i + 1) * 64)
                            S.op(S.pe, ("matmul", dict(out=PS[pz][0:64, ck], lhsT=A2T[:, ck], rhs=Pm[:, ck], start=True, stop=True)), reads=[BAT[nxt], BP], writes=[BPS[pz]])
                        S.op(S.dve, ("tensor_tensor", dict(out=Pm[:], in0=PS[pz][0:64, :], in1=Pm[:], op=ALU.add)), reads=[BPS[pz], BP], writes=[BP])
                        cur = nxt
                        yield
                    S.op(S.act, ("copy", dict(out=zto[gi][:], in_=Pm[:])), reads=[BP], writes=[Bzto[gi]])
                    S.dma(S.sp, ZT[(h, d)][cg], zto[gi][:], reads=[Bzto[gi]], writes=[BZT[(h, d)]])
                    yield
            def run_rr(gens):
                live = list(gens)
                while live:
                    nl = []
                    for gnr in live:
                        try:
                            next(gnr)
                            nl.append(gnr)
                        except StopIteration:
                            pass
                    live = nl
            streams = [(h, d) for h in range(n_g) for d in range(2)]
            NSTR = 4
            for s0 in range(0, len(streams), NSTR):
                grp = streams[s0:s0 + NSTR]
                nb_ = 8 // len(grp)
                run_rr([gdn_prep(h, d, f"c{h}{d}", k * nb_, (k + 1) * nb_) for k, (h, d) in enumerate(grp)])
          S.barrier()
        HQt = {}; HKt = {}; HQB = {}; HKD = {}
        BHQt = {}; BHKt = {}; BHQB = {}; BHKD = {}
        for h in range(n_h):
            for d in range(2):
                HQt[(h, d)] = scratch(f"HQt{h}{d}", [128, T], BF16); HKt[(h, d)] = scratch(f"HKt{h}{d}", [128, T], BF16)
                HQB[(h, d)] = scratch(f"HQB{h}{d}", [128, T], BF16); HKD[(h, d)] = scratch(f"HKD{h}{d}", [T, 128], BF16)
                BHQt[(h, d)] = Buf(); BHKt[(h, d)] = Buf(); BHQB[(h, d)] = Buf(); BHKD[(h, d)] = Buf()
                ALLOUT += [BHQt[(h, d)], BHKt[(h, d)], BHQB[(h, d)], BHKD[(h, d)]]
        if n_h:
          with contextlib.ExitStack() as ph:
            NL = n_h * 2
            lbr = S.sb("lbr", [128, NL * DEPTH_], F32, ph); Blb = Buf()
            S.dma(S.sp, lbr[:], hlb[:, :], writes=[Blb])
            lbv = lbr[:].rearrange("p (a l) -> p a l", l=DEPTH_)
            lw = S.sb("lw", [128, NL * 8], F32, ph)
            LW = lambda k: lw[:, k * NL:(k + 1) * NL]
            S.op(S.dve, ("tensor_copy", dict(out=LW(0), in_=lbv[:, :, 0])), reads=[Blb], writes=[Blb])
            for l in range(1, DEPTH_):
                S.op(S.dve, ("tensor_tensor", dict(out=LW(0), in0=LW(0), in1=lbv[:, :, l], op=ALU.max)), reads=[Blb], writes=[Blb])
            S.op(S.dve, ("tensor_tensor", dict(out=lbv, in0=lbv, in1=LW(0).unsqueeze(2).broadcast_to([128, NL, DEPTH_]), op=ALU.subtract)), reads=[Blb], writes=[Blb])
            S.op(S.act, ("activation", dict(out=lbr[:], in_=lbr[:], func=AF.Exp)), reads=[Blb], writes=[Blb])
            S.op(S.dve, ("tensor_reduce", dict(out=LW(1), in_=lbv, axis=AX.X, op=ALU.add)), reads=[Blb], writes=[Blb])
            S.op(S.dve, ("reciprocal", dict(out=LW(2), in_=LW(1))), reads=[Blb], writes=[Blb])
            S.op(S.dve, ("tensor_tensor", dict(out=lbv, in0=lbv, in1=LW(2).unsqueeze(2).broadcast_to([128, NL, DEPTH_]), op=ALU.mult)), reads=[Blb], writes=[Blb])
            cmk = S.sb("cmk", [128, DEPTH_], F32, ph)
            S.dma(S.sp, cmk[:], hcm[:, :], writes=[Blb])
            S.op(S.dve, ("tensor_scalar", dict(out=LW(3), in0=lbv[:, :, 0], scalar1=cmk[:, 0:1], scalar2=None, op0=ALU.mult)), reads=[Blb], writes=[Blb])
            for l in range(1, DEPTH_):
                S.op(S.dve, ("scalar_tensor_tensor", dict(out=LW(3), in0=lbv[:, :, l], scalar=cmk[:, l:l + 1], in1=LW(3), op0=ALU.mult, op1=ALU.add)), reads=[Blb], writes=[Blb])
            S.op(S.dve, ("tensor_tensor", dict(out=LW(4), in0=LW(3), in1=lbv[:, :, 0], op=ALU.subtract)), reads=[Blb], writes=[Blb])
            S.op(S.dve, ("tensor_scalar", dict(out=LW(5), in0=LW(4), scalar1=-1.0, scalar2=1.0, op0=ALU.mult, op1=ALU.add)), reads=[Blb], writes=[Blb])
            S.op(S.dve, ("tensor_scalar", dict(out=LW(6), in0=LW(5), scalar1=-1.0, scalar2=None, op0=ALU.mult)), reads=[Blb], writes=[Blb])
            xq = S.sb("hxq", [128, TB], F32, ph); Bxq = Buf()
            xf = S.sb("hxf", [128, TB], F32, ph); Bxf = Buf()
            sg_ = S.sb("hsg", [128, TB], F32, ph); Bsg = Buf()
            kin = S.sb("hkin", [128, TB], F32, ph); Bkin = Buf()
            ca = S.sb("hca", [128, TB], F32, ph); Bca = Buf()
            cb_ = S.sb("hcb", [128, TB], F32, ph); Bcb = Buf()
            w1 = S.sb("hw1", [128, TB], F32, ph); Bw1 = Buf()
            w2 = S.sb("hw2", [128, TB], F32, ph); Bw2 = Buf()
            hob = [S.sb(f"hob{i}", [128, TB], BF16, ph) for i in range(3)]; Bhob = [Buf(), Buf(), Buf()]
            tsb2 = [S.sb(f"htsb{i}", [128, 512], BF16, ph) for i in range(2)]; Btsb2 = [Buf(), Buf()]
            NCB = TB // 64
            c3 = lambda t_: t_[:].rearrange("p (n s) -> p n s", s=64)
            nts = 0
            for h in range(n_h):
                for d in range(2):
                    li = h * 2 + d
                    LB = lambda k: lw[:, k * NL + li:k * NL + li + 1]
                    for tb in range(NTB):
                        t0 = tb * TB
                        tsl = slice(t0, t0 + TB)
                        S.dma(S.sp, xq[:], FMs[f"hq{h}"][:, tsl], reads=[B_FM[f"hq{h}"]], writes=[Bxq])
                        S.dma(S.sp, xf[:], FMs[f"hf{d}{h}"][:, tsl], reads=[B_FM[f"hf{d}{h}"]], writes=[Bxf])
                        S.op(S.act, ("activation", dict(out=sg_[:], in_=xf[:], func=AF.Sigmoid)), reads=[Bxf], writes=[Bsg])
                        S.op(S.dve, ("tensor_scalar", dict(out=ca[:], in0=sg_[:], scalar1=LB(5), scalar2=LB(4), op0=ALU.mult, op1=ALU.add)), reads=[Bsg, Blb], writes=[Bca])
                        S.op(S.pool, ("tensor_scalar", dict(out=ca[:], in0=ca[:], scalar1=1.17549435e-38, scalar2=None, op0=ALU.max)), reads=[Bca], writes=[Bca])
                        S.op(S.act, ("activation", dict(out=ca[:], in_=ca[:], func=AF.Ln)), reads=[Bca], writes=[Bca])
                        S.op(S.dve, ("tensor_scalar", dict(out=kin[:], in0=sg_[:], scalar1=LB(6), scalar2=LB(5), op0=ALU.mult, op1=ALU.add)), reads=[Bsg, Blb], writes=[Bkin])
                        a, b, Ba, Bb = ca, cb_, Bca, Bcb
                        for sh in (1, 2, 4, 8, 16, 32):
                            if d == 0:
                                S.op(S.dve, ("tensor_tensor", dict(out=c3(b)[:, :, sh:], in0=c3(a)[:, :, sh:], in1=c3(a)[:, :, :64 - sh], op=ALU.add)), reads=[Ba], writes=[Bb])
                                S.op(S.pool, ("tensor_copy", dict(out=c3(b)[:, :, :sh], in_=c3(a)[:, :, :sh])), reads=[Ba], writes=[Bb])
                            else:
                                S.op(S.dve, ("tensor_tensor", dict(out=c3(b)[:, :, :64 - sh], in0=c3(a)[:, :, :64 - sh], in1=c3(a)[:, :, sh:], op=ALU.add)), reads=[Ba], writes=[Bb])
                                S.op(S.pool, ("tensor_copy", dict(out=c3(b)[:, :, 64 - sh:], in_=c3(a)[:, :, 64 - sh:])), reads=[Ba], writes=[Bb])
                            a, b, Ba, Bb = b, a, Bb, Ba
                        bc_, Bbc = a, Ba
                        lastc = 63 if d == 0 else 0
                        S.op(S.act, ("activation", dict(out=hsm[(h, d)]["el"][:, tb * NCB:(tb + 1) * NCB], in_=c3(bc_)[:, :, lastc], func=AF.Exp)), reads=[Bbc], writes=[hsm[(h, d)]["B"]])
                        S.op(S.dve, ("tensor_tensor", dict(out=c3(w1), in0=c3(bc_), in1=c3(bc_)[:, :, 32:33].broadcast_to([128, NCB, 64]), op=ALU.subtract)), reads=[Bbc], writes=[Bw1])
                        S.op(S.act, ("activation", dict(out=w2[:], in_=w1[:], func=AF.Exp)), reads=[Bw1], writes=[Bw2])
                        S.op(S.dve, ("tensor_tensor", dict(out=hob[0][:], in0=xq[:], in1=w2[:], op=ALU.mult)), reads=[Bxq, Bw2], writes=[Bhob[0]])
                        S.dma(S.sp, HQt[(h, d)][:, tsl], hob[0][:], reads=[Bhob[0]], writes=[BHQt[(h, d)]])
                        S.op(S.act, ("activation", dict(out=w2[:], in_=w1[:], func=AF.Exp, scale=-1.0)), reads=[Bw1, Bhob[0]], writes=[Bw2])
                        S.op(S.dve, ("tensor_tensor", dict(out=hob[1][:], in0=kin[:], in1=w2[:], op=ALU.mult)), reads=[Bkin, Bw2], writes=[Bhob[1]])
                        S.dma(S.sp, HKt[(h, d)][:, tsl], hob[1][:], reads=[Bhob[1]], writes=[BHKt[(h, d)]])
                        S.op(S.act, ("activation", dict(out=w2[:], in_=bc_[:], func=AF.Exp)), reads=[Bbc, Bhob[1]], writes=[Bw2])
                        S.op(S.dve, ("tensor_tensor", dict(out=hob[2][:], in0=xq[:], in1=w2[:], op=ALU.mult)), reads=[Bxq, Bw2], writes=[Bhob[2]])
                        S.dma(S.sp, HQB[(h, d)][:, tsl], hob[2][:], reads=[Bhob[2]], writes=[BHQB[(h, d)]])
                        S.op(S.dve, ("tensor_tensor", dict(out=c3(w1), in0=c3(bc_)[:, :, lastc:lastc + 1].broadcast_to([128, NCB, 64]), in1=c3(bc_), op=ALU.subtract)), reads=[Bbc, Bw1], writes=[Bw1])
                        S.op(S.act, ("activation", dict(out=w2[:], in_=w1[:], func=AF.Exp)), reads=[Bw1, Bhob[2]], writes=[Bw2])
                        S.op(S.dve, ("tensor_tensor", dict(out=w1[:], in0=kin[:], in1=w2[:], op=ALU.mult)), reads=[Bkin, Bw2], writes=[Bw1])
                        for sbk in range(TB // 512):
                            pb = nextpb()
                            for jj in range(4):
                                c0 = sbk * 512 + jj * 128
                                S.op(S.pe, ("transpose", dict(out=PS[pb][:, jj * 128:(jj + 1) * 128], in_=w1[:, c0:c0 + 128], identity=ident)), reads=[Bc, Bw1], writes=[BPS[pb]])
                            ts = nts % 2; nts += 1
                            tt0 = t0 + sbk * 512
                            S.op(S.act, ("copy", dict(out=tsb2[ts][:], in_=PS[pb][:, :])), reads=[BPS[pb]], writes=[Btsb2[ts]])
                            S.dma(S.sp, HKD[(h, d)][tt0:tt0 + 512, :].rearrange("(j p) e -> p j e", p=128), tsb2[ts][:].rearrange("p (j e) -> p j e", e=128),
                                  reads=[Btsb2[ts]], writes=[BHKD[(h, d)]])
          S.barrier()
        OH = {}; BOH = {}
        for h in range(n_h):
            for d in range(2):
                OH[(h, d)] = scratch(f"OH{h}{d}", [T, 128], F32); BOH[(h, d)] = Buf()
        with contextlib.ExitStack() as ph:
            scans = []
            bank = [0]
            NBUF_ = 2
            def gdn_scan(h, d, tag):
                sm = gsm[(h, d)]
                pA = bank[0]; pB = bank[0] + 1; bank[0] += 2
                Sf = S.sb(f"Sf{tag}", [128, 128], F32, ph); BSf = Buf()
                Sb = S.sb(f"Sb{tag}", [128, 128], BF16, ph); BSb = Buf()
                kg2 = [S.sb(f"dk{tag}{i}", [128, 512], BF16, ph) for i in range(NBUF_)]
                qg2 = [S.sb(f"dq{tag}{i}", [128, 512], BF16, ph) for i in range(NBUF_)]
                kt2 = [S.sb(f"dkt{tag}{i}", [64, 8 * 128], BF16, ph) for i in range(NBUF_)]
                vt2 = [S.sb(f"dvt{tag}{i}", [64, 8 * 128], F32, ph) for i in range(NBUF_)]
                zt2 = [S.sb(f"dz{tag}{i}", [64, 512], BF16, ph) for i in range(NBUF_)]
                qk2 = [S.sb(f"dqk{tag}{i}", [64, 512], BF16, ph) for i in range(NBUF_)]
                og2 = [S.sb(f"dog{tag}{i}", [64, 8 * 128], F32, ph) for i in range(NBUF_)]
                Bin = [Buf() for _ in range(NBUF_)]; Bog = [Buf() for _ in range(NBUF_)]
                Rb = S.sb(f"dR{tag}", [64, 128], BF16, ph); BR = Buf()
                vn = S.sb(f"dvn{tag}", [64, 128], BF16, ph); Bvn = Buf()
                vs = S.sb(f"dvs{tag}", [64, 128], BF16, ph); Bvs = Buf()
                S.op(S.pool, ("memset", dict(ap=Sf[:], constant=0.0)), writes=[BSf])
                S.op(S.pool, ("memset", dict(ap=Sb[:], constant=0.0)), writes=[BSb])
                yield
                groups = list(range(NG)) if d == 0 else list(range(NG - 1, -1, -1))
                def gload(gidx):
                    cg = groups[gidx]; gi = gidx % NBUF_
                    tk = slice(cg * 512, (cg + 1) * 512)
                    S.dma(S.sp, kg2[gi][:], KhT[h][:, tk], reads=[BKhT[h]], writes=[Bin[gi]])
                    S.dma(S.sp, qg2[gi][:], QG[(h, d)][:, tk], reads=[BQG[(h, d)]], writes=[Bin[gi]])
                    S.dma(S.sp, kt2[gi][:].rearrange("p (i e) -> p i e", e=128), Ktok[h][tk, :].rearrange("(i s) e -> s i e", s=64), reads=[BKtok[h]], writes=[Bin[gi]])
                    S.dma(S.sp, vt2[gi][:].rearrange("p (i e) -> p i e", e=128), Vtok[h][tk, :].rearrange("(i s) e -> s i e", s=64), reads=[BVtok[h]], writes=[Bin[gi]])
                    S.dma(S.sp, zt2[gi][:], ZT[(h, d)][cg], reads=[BZT[(h, d)]], writes=[Bin[gi]])
                    S.dma(S.sp, qk2[gi][:], QKM[(h, d)][cg], reads=[BQKM[(h, d)]], writes=[Bin[gi]])
                gload(0)
                yield
                for gidx, cg in enumerate(groups):
                    gi = gidx % NBUF_
                    tk = slice(cg * 512, (cg + 1) * 512)
                    if gidx + 1 < len(groups):
                        gload(gidx + 1)
                    yield
                    order = range(8) if d == 0 else range(7, -1, -1)
                    for i in order:
                        n = cg * 8 + i
                        ck = slice(i * 64, (i + 1) * 64)
                        ce = slice(i * 128, (i + 1) * 128)
                        S.op(S.pe, ("matmul", dict(out=PS[pA][0:64, 0:128], lhsT=kg2[gi][:, ck], rhs=Sb[:], start=True, stop=True)), reads=[Bin[gi], BSb], writes=[BPS[pA]])
                        S.op(S.dve, ("scalar_tensor_tensor", dict(out=Rb[:], in0=PS[pA][0:64, 0:128], scalar=sm["neg"][:, n:n + 1], in1=vt2[gi][:, ce], op0=ALU.mult, op1=ALU.add)),
                             reads=[BPS[pA], sm["B"], Bin[gi]], writes=[BR])
                        yield
                        S.op(S.pe, ("matmul", dict(out=PS[pA][0:64, 128:256], lhsT=zt2[gi][:, ck], rhs=Rb[:], start=True, stop=True)), reads=[Bin[gi], BR], writes=[BPS[pA]])
                        S.op(S.act, ("activation", dict(out=vn[:], in_=PS[pA][0:64, 128:256], func=AF.Copy, scale=sm["b"][:, n:n + 1])), reads=[BPS[pA], sm["B"]], writes=[Bvn])
                        S.op(S.dve, ("tensor_scalar", dict(out=vs[:], in0=PS[pA][0:64, 128:256], scalar1=sm["bkd"][:, n:n + 1], scalar2=None, op0=ALU.mult)), reads=[BPS[pA], sm["B"]], writes=[Bvs])
                        yield
                        S.op(S.pe, ("matmul", dict(out=PS[pB][0:64, 0:128], lhsT=qg2[gi][:, ck], rhs=Sb[:], start=True, stop=False)), reads=[Bin[gi], BSb], writes=[BPS[pB]])
                        S.op(S.pe, ("matmul", dict(out=PS[pB][0:64, 0:128], lhsT=qk2[gi][:, ck], rhs=vn[:], start=False, stop=True)), reads=[Bin[gi], Bvn], writes=[BPS[pB]])
                        S.op(S.act, ("copy", dict(out=og2[gi][:, ce], in_=PS[pB][0:64, 0:128])), reads=[BPS[pB]], writes=[Bog[gi]])
                        S.op(S.pe, ("matmul", dict(out=PS[pB][:, 128:256], lhsT=kt2[gi][:, ce], rhs=vs[:], start=True, stop=True)), reads=[Bin[gi], Bvs], writes=[BPS[pB]])
                        S.op(S.dve, ("scalar_tensor_tensor", dict(out=Sf[:], in0=Sf[:], scalar=sm["gl"][:, n:n + 1], in1=PS[pB][:, 128:256], op0=ALU.mult, op1=ALU.add)),
                             reads=[BSf, sm["B"], BPS[pB]], writes=[BSf])
                        S.op(S.act, ("copy", dict(out=Sb[:], in_=Sf[:])), reads=[BSf], writes=[BSb])
                        yield
                    S.dma(S.sp, OG[(h, d)][tk, :].rearrange("(i s) e -> s i e", s=64), og2[gi][:].rearrange("p (i e) -> p i e", e=128), reads=[Bog[gi]], writes=[BOG[(h, d)]])
                    yield

            def hgrn_scan(h, d, tag):
                sm = hsm[(h, d)]
                pA = bank[0]; pB = bank[0] + 1; bank[0] += 2
                vc = n_g * 128 + h * 256
                Sf = S.sb(f"Sf{tag}", [128, 128], F32, ph); BSf = Buf()
                Sb = S.sb(f"Sb{tag}", [128, 128], BF16, ph); BSb = Buf()
                qt2 = [S.sb(f"hq{tag}{i}", [128, 512], BF16, ph) for i in range(NBUF_)]
                kt2 = [S.sb(f"hk{tag}{i}", [128, 512], BF16, ph) for i in range(NBUF_)]
                qb2 = [S.sb(f"hb{tag}{i}", [128, 512], BF16, ph) for i in range(NBUF_)]
                kd2 = [S.sb(f"hd{tag}{i}", [64, 8 * 128], BF16, ph) for i in range(NBUF_)]
                v2 = [S.sb(f"hv{tag}{i}", [64, 8 * 128], BF16, ph) for i in range(NBUF_)]
                og2 = [S.sb(f"ho{tag}{i}", [64, 8 * 128], F32, ph) for i in range(NBUF_)]
                am2 = [S.sb(f"ha{tag}{i}", [64, 512], BF16, ph) for i in range(NBUF_)]
                Bin = [Buf() for _ in range(NBUF_)]; Bog = [Buf() for _ in range(NBUF_)]; Bam = [Buf() for _ in range(NBUF_)]
                S.op(S.pool, ("memset", dict(ap=Sf[:], constant=0.0)), writes=[BSf])
                S.op(S.pool, ("memset", dict(ap=Sb[:], constant=0.0)), writes=[BSb])
                yield
                groups = list(range(NG)) if d == 0 else list(range(NG - 1, -1, -1))
                def hload(gidx):
                    cg = groups[gidx]; gi = gidx % NBUF_
                    tk = slice(cg * 512, (cg + 1) * 512)
                    S.dma(S.sp, qt2[gi][:], HQt[(h, d)][:, tk], reads=[BHQt[(h, d)]], writes=[Bin[gi]])
                    S.dma(S.sp, kt2[gi][:], HKt[(h, d)][:, tk], reads=[BHKt[(h, d)]], writes=[Bin[gi]])
                    S.dma(S.sp, qb2[gi][:], HQB[(h, d)][:, tk], reads=[BHQB[(h, d)]], writes=[Bin[gi]])
                    S.dma(S.sp, kd2[gi][:].rearrange("p (i e) -> p i e", e=128), HKD[(h, d)][tk, :].rearrange("(i s) e -> s i e", s=64), reads=[BHKD[(h, d)]], writes=[Bin[gi]])
                    S.dma(S.pool, v2[gi][:].rearrange("p (i e) -> p i e", e=128), TMs[tk, vc:vc + 128].rearrange("(i s) e -> s i e", s=64), reads=[B_TM], writes=[Bin[gi]])
                hload(0)
                yield
                for gidx, cg in enumerate(groups):
                    gi = gidx % NBUF_
                    tk = slice(cg * 512, (cg + 1) * 512)
                    if gidx + 1 < len(groups):
                        hload(gidx + 1)
                    yield
                    for i in range(8):
                        ck = slice(i * 64, (i + 1) * 64)
                        S.op(S.pe, ("matmul", dict(out=PS[pA][0:64, ck], lhsT=kt2[gi][:, ck], rhs=qt2[gi][:, ck], start=True, stop=True)), reads=[Bin[gi]], writes=[BPS[pA]])
                    S.op(S.dve, ("tensor_tensor", dict(out=v8(am2[gi][:]), in0=v8(PS[pA][0:64, :]), in1=bc8(TRI(d)), op=ALU.mult)), reads=[BPS[pA], Bc], writes=[Bam[gi]])
                    yield
                    order = range(8) if d == 0 else range(7, -1, -1)
                    for i in order:
                        n = cg * 8 + i
                        ck = slice(i * 64, (i + 1) * 64)
                        ce = slice(i * 128, (i + 1) * 128)
                        S.op(S.pe, ("matmul", dict(out=PS[pB][0:64, 0:128], lhsT=qb2[gi][:, ck], rhs=Sb[:], start=True, stop=False)), reads=[Bin[gi], BSb], writes=[BPS[pB]])
                        S.op(S.pe, ("matmul", dict(out=PS[pB][0:64, 0:128], lhsT=am2[gi][:, ck], rhs=v2[gi][:, ce], start=False, stop=True)), reads=[Bin[gi], Bam[gi]], writes=[BPS[pB]])
                        S.op(S.act, ("copy", dict(out=og2[gi][:, ce], in_=PS[pB][0:64, 0:128])), reads=[BPS[pB]], writes=[Bog[gi]])
                        S.op(S.pe, ("matmul", dict(out=PS[pB][:, 128:256], lhsT=kd2[gi][:, ce], rhs=v2[gi][:, ce], start=True, stop=True)), reads=[Bin[gi]], writes=[BPS[pB]])
                        S.op(S.dve, ("scalar_tensor_tensor", dict(out=Sf[:], in0=Sf[:], scalar=sm["el"][:, n:n + 1], in1=PS[pB][:, 128:256], op0=ALU.mult, op1=ALU.add)),
                             reads=[BSf, sm["B"], BPS[pB]], writes=[BSf])
                        S.op(S.act, ("copy", dict(out=Sb[:], in_=Sf[:])), reads=[BSf], writes=[BSb])
                        yield
                    S.dma(S.sp, OH[(h, d)][tk, :].rearrange("(i s) e -> s i e", s=64), og2[gi][:].rearrange("p (i e) -> p i e", e=128), reads=[Bog[gi]], writes=[BOH[(h, d)]])
                    yield

            def run_scans(scans):
                live = list(scans)
                while live:
                    nxt_live = []
                    for gnr in live:
                        try:
                            next(gnr)
                            nxt_live.append(gnr)
                        except StopIteration:
                            pass
                    live = nxt_live
            run_scans([gdn_scan(h, d, f"g{h}{d}") for h in range(n_g) for d in range(2)])
        S.barrier()
        with contextlib.ExitStack() as ph:
            bank[0] = 0
            run_scans([hgrn_scan(h, d, f"h{h}{d}") for h in range(n_h) for d in range(2)])
        S.barrier()
        NOUT = (n_g + n_h) * 128
        omix = nc.dram_tensor("omix", [T, NOUT], BF16, kind="ExternalOutput").ap()
        Bomix = Buf(); ALLOUT.append(Bomix)
        with contextlib.ExitStack() as ph:
            nwt = S.sb("nwt", [128, 256], F32, ph); Bnw = Buf()
            S.dma(S.sp, nwt[:], normw[:, :], writes=[Bnw])
            JT = min(8, T // 128)
            of_ = [S.sb(f"e_of{i}", [128, JT * 128], F32, ph) for i in range(2)]
            ob_ = [S.sb(f"e_ob{i}", [128, JT * 128], F32, ph) for i in range(2)]
            zz_ = [S.sb(f"e_z{i}", [128, JT * 128], F32, ph) for i in range(2)]
            Bei = [Buf(), Buf()]
            sqe = S.sb("e_sq", [128, JT * 128], F32, ph); Bsqe = Buf()
            sse = S.sb("e_ss", [128, JT], F32, ph); Bsse = Buf()
            yo = [S.sb(f"e_y{i}", [128, JT * 128], BF16, ph) for i in range(2)]; Byo = [Buf(), Buf()]
            ne = 0
            jobs = [("g", h) for h in range(n_g)] + [("h", h) for h in range(n_h)]
            for ji, (kind, h) in enumerate(jobs):
                if kind == "g":
                    Of, Ob, BOf, BOb = OG[(h, 0)], OG[(h, 1)], BOG[(h, 0)], BOG[(h, 1)]
                    zc = h * 128; wcol = 0
                else:
                    Of, Ob, BOf, BOb = OH[(h, 0)], OH[(h, 1)], BOH[(h, 0)], BOH[(h, 1)]
                    zc = n_g * 128 + h * 256 + 128; wcol = 128
                v3 = lambda t_: t_[:].rearrange("p (j e) -> p j e", e=128)
                for t0 in range(0, T, JT * 128):
                    e = ne % 2; ne += 1
                    tsl = slice(t0, t0 + JT * 128)
                    S.dma(S.sp, v3(of_[e]), Of[tsl, :].rearrange("(j p) e -> p j e", p=128), reads=[BOf], writes=[Bei[e]])
                    S.dma(S.sp, v3(ob_[e]), Ob[tsl, :].rearrange("(j p) e -> p j e", p=128), reads=[BOb], writes=[Bei[e]])
                    S.dma(S.sp, v3(zz_[e]), TMs[tsl, zc:zc + 128].rearrange("(j p) e -> p j e", p=128), reads=[B_TM], writes=[Bei[e]])
                    S.op(S.dve, ("tensor_tensor", dict(out=of_[e][:], in0=of_[e][:], in1=ob_[e][:], op=ALU.add)), reads=[Bei[e]], writes=[Bei[e]])
                    S.op(S.act, ("activation", dict(out=sqe[:], in_=of_[e][:], func=AF.Square)), reads=[Bei[e]], writes=[Bsqe])
                    S.op(S.dve, ("tensor_reduce", dict(out=sse[:], in_=v3(sqe), axis=AX.X, op=ALU.add)), reads=[Bsqe], writes=[Bsse])
                    S.op(S.dve, ("tensor_scalar", dict(out=sse[:], in0=sse[:], scalar1=1.0 / 128, scalar2=1e-6, op0=ALU.mult, op1=ALU.add)), reads=[Bsse], writes=[Bsse])
                    S.op(S.act, ("sqrt", dict(out=sse[:], in_=sse[:])), reads=[Bsse], writes=[Bsse])
                    S.op(S.dve, ("reciprocal", dict(out=sse[:], in_=sse[:])), reads=[Bsse], writes=[Bsse])
                    S.op(S.dve, ("tensor_tensor", dict(out=v3(of_[e]), in0=v3(of_[e]), in1=sse[:].unsqueeze(2).broadcast_to([128, JT, 128]), op=ALU.mult)), reads=[Bei[e], Bsse], writes=[Bei[e]])
                    S.op(S.pool, ("tensor_tensor", dict(out=v3(of_[e]), in0=v3(of_[e]), in1=nwt[:, wcol:wcol + 128].unsqueeze(1).broadcast_to([128, JT, 128]), op=ALU.mult)), reads=[Bei[e], Bnw], writes=[Bei[e]])
                    S.op(S.act, ("activation", dict(out=zz_[e][:], in_=zz_[e][:], func=AF.Silu)), reads=[Bei[e]], writes=[Bei[e]])
                    S.op(S.dve, ("tensor_tensor", dict(out=yo[e][:], in0=of_[e][:], in1=zz_[e][:], op=ALU.mult)), reads=[Bei[e]], writes=[Byo[e]])
                    S.dma(S.sp, omix[tsl, ji * 128:(ji + 1) * 128].rearrange("(j p) e -> p j e", p=128), v3(yo[e]), reads=[Byo[e]], writes=[Bomix])

        S.finish([b for b in ALLOUT])
        S.emit()
    return nc, dbg


D = 2048
KC = 16
NE = 16


def build_tok(Tc, mode, KCH=0, last=False):
    nc = bass.Bass("TRN2", target_bir_lowering=False)
    NT = Tc // 128
    h_in = nc.dram_tensor("h", [Tc, D], F32, kind="ExternalInput").ap()
    nwb = nc.dram_tensor("nwb", [128, D], F32, kind="ExternalInput").ap()
    if mode == "O":
        oT = nc.dram_tensor("oT", [D, Tc], BF16, kind="ExternalInput").ap()
        w_out = nc.dram_tensor("w_out", [D, D], F32, kind="ExternalInput").ap()
        w_r = nc.dram_tensor("w_r", [D, NE], F32, kind="ExternalInput").ap()
        ident_in = nc.dram_tensor("ident", [128, 128], F32, kind="ExternalInput").ap()
        u_out = nc.dram_tensor("u_out", [Tc, D], BF16, kind="ExternalOutput").ap()
        p_out = nc.dram_tensor("p_out", [Tc, NE], F32, kind="ExternalOutput").ap()
    elif mode == "C":
        oT = nc.dram_tensor("oT", [D, Tc], BF16, kind="ExternalInput").ap()
        w_out = nc.dram_tensor("w_out", [D, D], F32, kind="ExternalInput").ap()
        rows = nc.dram_tensor("rows", [NT, max(KCH, 1), 128, D], BF16, kind="ExternalInput").ap()
        idx = nc.dram_tensor("idx", [128, NT * max(KCH, 1)], F32, kind="ExternalInput").ap()
        iota_in = nc.dram_tensor("iota", [128, 128], F32, kind="ExternalInput").ap()
        if last:
            f_out = nc.dram_tensor("f_out", [Tc, D], F32, kind="ExternalOutput").ap()
        else:
            h_out = nc.dram_tensor("h_out", [Tc, D], F32, kind="ExternalOutput").ap()
            u_out = nc.dram_tensor("u_out", [Tc, D], BF16, kind="ExternalOutput").ap()
    else:
        u_out = nc.dram_tensor("u_out", [Tc, D], BF16, kind="ExternalOutput").ap()
    with contextlib.ExitStack() as st:
        S = Sched(nc, st)
        PS = [S.ps(f"ps{i}", [128, 512]) for i in range(8)]
        BPS = [Buf(f"ps{i}", excl=True) for i in range(8)]
        Bout = Buf()
        nw = S.sb("nw", [128, D]); Bnw = Buf()
        S.dma(S.sp, nw[:], nwb[:, :], writes=[Bnw])
        ht = [S.sb(f"ht{i}", [128, D]) for i in range(2)]; Bht = [Buf(), Buf()]
        junk = S.sb("junk", [128, D]); Bj = Buf()
        ss = [S.sb(f"ss{i}", [128, 8]) for i in range(2)]; Bss = [Buf(), Buf()]
        if mode == "O":
            wo = S.sb("wo", [128, KC, D], BF16); Bwo = Buf()
            for kc in range(KC):
                S.dma(S.pool, wo[:, kc, :], w_out[kc * 128:(kc + 1) * 128, :], writes=[Bwo])
            wr = S.sb("wr", [128, KC, NE]); Bwr = Buf()
            S.dma(S.sp, wr[:], w_r.rearrange("(kc p) e -> p kc e", p=128), writes=[Bwr])
            idt = S.sb("idt", [128, 128]); Bid = Buf()
            S.dma(S.sp, idt[:], ident_in[:, :], writes=[Bid])
            ot = [S.sb(f"ot{i}", [128, KC, 512], BF16) for i in range(2)]; Bot = [Buf(), Buf()]
            uf = S.sb("uf", [128, D]); Buf_ = Buf()
            uT = S.sb("uT", [128, KC, 128]); BuT = Buf()
            lg = [S.sb(f"lg{i}", [128, 32]) for i in range(2)]; Blg = [Buf(), Buf()]
            oTv = oT.rearrange("(kc p) t -> p kc t", p=128)
        if mode == "C":
            wo = S.sb("wo", [128, KC, D], BF16); Bwo = Buf()
            for kc in range(KC):
                S.dma(S.pool, wo[:, kc, :], w_out[kc * 128:(kc + 1) * 128, :], writes=[Bwo])
            ot = [S.sb(f"ot{i}", [128, KC, 512], BF16) for i in range(2)]; Bot = [Buf(), Buf()]
            oTv = oT.rearrange("(kc p) t -> p kc t", p=128)
            io = S.sb("io", [128, 128]); Bio = Buf()
            S.dma(S.sp, io[:], iota_in[:, :], writes=[Bio])
            ix = S.sb("ix", [128, NT * max(KCH, 1)]); Bix = Buf()
            S.dma(S.sp, ix[:], idx[:, :], writes=[Bix])
            rw = [S.sb(f"rw{i}", [128, D], BF16) for i in range(3)]; Brw = [Buf(), Buf(), Buf()]
            Pm = [S.sb(f"Pm{i}", [128, 128], BF16) for i in range(3)]; BPm = [Buf(), Buf(), Buf()]
        ub = [S.sb(f"ub{i}", [128, D], BF16) for i in range(2)]; Bub = [Buf(), Buf()]
        fo = S.sb("fo", [128, D]) if (mode == "C" and last) else None; Bfo = Buf()
        nrw = 0
        for i in range(NT):
            k = i % 2
            tsl = slice(i * 128, (i + 1) * 128)
            S.dma(S.sp, ht[k][:], h_in[tsl, :], writes=[Bht[k]])
            if mode == "O":
                if i % 4 == 0:
                    ob = (i // 4) % 2
                    S.dma(S.sp, ot[ob][:], oTv[:, :, i * 128:i * 128 + 512], writes=[Bot[ob]])
                tt = i % 4
                for nb in range(4):
                    for kc in range(KC):
                        S.op(S.pe, ("matmul", dict(out=PS[nb][:, :], lhsT=ot[ob][:, kc, tt * 128:(tt + 1) * 128], rhs=wo[:, kc, nb * 512:(nb + 1) * 512],
                                                   start=(kc == 0), stop=(kc == KC - 1))), reads=[Bot[ob], Bwo], writes=[BPS[nb]])
                for nb in range(4):
                    sl = slice(nb * 512, (nb + 1) * 512)
                    S.op(S.dve, ("tensor_tensor", dict(out=ht[k][:, sl], in0=ht[k][:, sl], in1=PS[nb][:, :], op=ALU.add)), reads=[BPS[nb], Bht[k]], writes=[Bht[k]])
            if mode == "C":
                if True:
                    if i % 4 == 0:
                        ob = (i // 4) % 2
                        S.dma(S.sp, ot[ob][:], oTv[:, :, i * 128:i * 128 + 512], writes=[Bot[ob]])
                    tt = i % 4
                    rws = []
                    for j in range(KCH):
                        r = nrw % 3; nrw += 1
                        S.dma(S.sp if j % 2 == 0 else S.act, rw[r][:], rows[i, j], writes=[Brw[r]])
                        S.op(S.dve, ("tensor_scalar", dict(out=Pm[r][:], in0=io[:], scalar1=ix[:, i * KCH + j:i * KCH + j + 1], scalar2=None, op0=ALU.is_equal)),
                             reads=[Bio, Bix], writes=[BPm[r]])
                        rws.append(r)
                    for nb in range(4):
                        for kc in range(KC):
                            S.op(S.pe, ("matmul", dict(out=PS[nb][:, :], lhsT=ot[ob][:, kc, tt * 128:(tt + 1) * 128], rhs=wo[:, kc, nb * 512:(nb + 1) * 512],
                                                       start=(kc == 0), stop=(kc == KC - 1 and KCH == 0))), reads=[Bot[ob], Bwo], writes=[BPS[nb]])
                        for j, r in enumerate(rws):
                            S.op(S.pe, ("matmul", dict(out=PS[nb][:, :], lhsT=Pm[r][:], rhs=rw[r][:, nb * 512:(nb + 1) * 512], start=False, stop=(j == KCH - 1))),
                                 reads=[BPm[r], Brw[r]], writes=[BPS[nb]])
                    for nb in range(4):
                        sl = slice(nb * 512, (nb + 1) * 512)
                        S.op(S.dve, ("tensor_tensor", dict(out=ht[k][:, sl], in0=ht[k][:, sl], in1=PS[nb][:, :], op=ALU.add)), reads=[BPS[nb], Bht[k]], writes=[Bht[k]])
                if not last:
                    S.dma(S.sp, h_out[tsl, :], ht[k][:], reads=[Bht[k]], writes=[Bout])
            S.op(S.act, ("activation", dict(out=junk[:], in_=ht[k][:], func=AF.Square, accum_out=ss[k][:, 0:1])), reads=[Bht[k]], writes=[Bj, Bss[k]])
            S.op(S.dve, ("tensor_scalar", dict(out=ss[k][:, 1:2], in0=ss[k][:, 0:1], scalar1=1.0 / D, scalar2=1e-6, op0=ALU.mult, op1=ALU.add)), reads=[Bss[k]], writes=[Bss[k]])
            S.op(S.act, ("sqrt", dict(out=ss[k][:, 2:3], in_=ss[k][:, 1:2])), reads=[Bss[k]], writes=[Bss[k]])
            S.op(S.dve, ("reciprocal", dict(out=ss[k][:, 3:4], in_=ss[k][:, 2:3])), reads=[Bss[k]], writes=[Bss[k]])
            if mode == "C" and last:
                S.op(S.dve, ("scalar_tensor_tensor", dict(out=fo[:], in0=ht[k][:], scalar=ss[k][:, 3:4], in1=nw[:], op0=ALU.mult, op1=ALU.mult)), reads=[Bht[k], Bss[k], Bnw], writes=[Bfo])
                S.dma(S.sp, f_out[tsl, :], fo[:], reads=[Bfo], writes=[Bout])
            elif mode == "O":
                S.op(S.dve, ("scalar_tensor_tensor", dict(out=uf[:], in0=ht[k][:], scalar=ss[k][:, 3:4], in1=nw[:], op0=ALU.mult, op1=ALU.mult)), reads=[Bht[k], Bss[k], Bnw], writes=[Buf_])
                S.op(S.act, ("copy", dict(out=ub[k][:], in_=uf[:])), reads=[Buf_], writes=[Bub[k]])
                S.dma(S.sp, u_out[tsl, :], ub[k][:], reads=[Bub[k]], writes=[Bout])
                for q4 in range(4):
                    pb = 4 + q4
                    for jj in range(4):
                        kc = q4 * 4 + jj
                        S.op(S.pe, ("transpose", dict(out=PS[pb][:, jj * 128:(jj + 1) * 128], in_=uf[:, kc * 128:(kc + 1) * 128], identity=idt[:])), reads=[Buf_, Bid], writes=[BPS[pb]])
                    dst = uT[:, q4 * 4:(q4 + 1) * 4, :]
                    srcv = PS[pb][:, :].rearrange("p (a b) -> p a b", b=128)
                    if q4 % 2 == 0:
                        S.op(S.act, ("copy", dict(out=dst, in_=srcv)), reads=[BPS[pb]], writes=[BuT])
                    else:
                        S.op(S.dve, ("tensor_copy", dict(out=dst, in_=srcv)), reads=[BPS[pb]], writes=[BuT])
                for kc in range(KC):
                    S.op(S.pe, ("matmul", dict(out=PS[4][:, 0:NE], lhsT=uT[:, kc, :], rhs=wr[:, kc, :], start=(kc == 0), stop=(kc == KC - 1))), reads=[BuT, Bwr], writes=[BPS[4]])
                l = lg[k]
                S.op(S.dve, ("tensor_reduce", dict(out=l[:, 16:17], in_=PS[4][:, 0:NE], axis=AX.X, op=ALU.max)), reads=[BPS[4]], writes=[Blg[k]])
                S.op(S.dve, ("tensor_scalar", dict(out=l[:, 17:18], in0=l[:, 16:17], scalar1=-1.0, scalar2=None, op0=ALU.mult)), reads=[Blg[k]], writes=[Blg[k]])
                S.op(S.act, ("activation", dict(out=l[:, 0:NE], in_=PS[4][:, 0:NE], func=AF.Exp, bias=l[:, 17:18], accum_out=l[:, 18:19])), reads=[BPS[4], Blg[k]], writes=[Blg[k]])
                S.op(S.dve, ("reciprocal", dict(out=l[:, 19:20], in_=l[:, 18:19])), reads=[Blg[k]], writes=[Blg[k]])
                S.op(S.dve, ("tensor_scalar", dict(out=l[:, 0:NE], in0=l[:, 0:NE], scalar1=l[:, 19:20], scalar2=None, op0=ALU.mult)), reads=[Blg[k]], writes=[Blg[k]])
                S.dma(S.sp, p_out[tsl, :], l[:, 0:NE], reads=[Blg[k]], writes=[Bout])
            else:
                S.op(S.dve, ("scalar_tensor_tensor", dict(out=ub[k][:], in0=ht[k][:], scalar=ss[k][:, 3:4], in1=nw[:], op0=ALU.mult, op1=ALU.mult)), reads=[Bht[k], Bss[k], Bnw], writes=[Bub[k]])
                S.dma(S.sp, u_out[tsl, :], ub[k][:], reads=[Bub[k]], writes=[Bout])
        S.finish([Bout])
        S.emit()
    return nc


D = 2048
KC = 16


def build_topk(NBE, J, KCAP, iters=36):
    nc = bass.Bass("TRN2", target_bir_lowering=False)
    P_in = nc.dram_tensor("P", [128, NBE * J], F32, kind="ExternalInput").ap()
    ones_in = nc.dram_tensor("ones", [128, 128], F32, kind="ExternalInput").ap()
    m_out = nc.dram_tensor("mask", [128, NBE * J], F32, kind="ExternalOutput").ap()
    c_out = nc.dram_tensor("cnt", [128, NBE], F32, kind="ExternalOutput").ap()
    with contextlib.ExitStack() as st:
        S = Sched(nc, st)
        PS = [S.ps(f"ps{i}", [128, 512]) for i in range(2)]
        BPS = [Buf(f"ps{i}", excl=True) for i in range(2)]
        P = S.sb("Psb", [128, NBE * J]); BP = Buf()
        S.dma(S.sp, P[:], P_in[:, :], writes=[BP])
        on = S.sb("on", [128, 128]); Bon = Buf()
        S.dma(S.sp, on[:], ones_in[:, :], writes=[Bon])
        cmp_ = S.sb("cmp", [128, NBE * J]); Bcmp = Buf()
        w = S.sb("w", [128, 8 * NBE]); Bw = Buf()
        W = lambda k: w[:, k * NBE:(k + 1) * NBE]
        lo, hi, mid, cnt, ge, t1, t2 = (W(k) for k in range(7))
        P3 = P[:].rearrange("p (a j) -> p a j", j=J)
        C3 = cmp_[:].rearrange("p (a j) -> p a j", j=J)
        S.op(S.pool, ("memset", dict(ap=lo, constant=0.0)), writes=[Bw])
        S.op(S.pool, ("memset", dict(ap=hi, constant=2.0)), writes=[Bw])
        def count(thr, pb):
            S.op(S.dve, ("tensor_tensor", dict(out=C3, in0=P3, in1=thr.unsqueeze(2).broadcast_to([128, NBE, J]), op=ALU.is_ge)), reads=[BP, Bw], writes=[Bcmp])
            S.op(S.dve, ("tensor_reduce", dict(out=cnt, in_=C3, axis=AX.X, op=ALU.add)), reads=[Bcmp], writes=[Bw])
            S.op(S.pe, ("matmul", dict(out=PS[pb][:, 0:NBE], lhsT=on[:], rhs=cnt, start=True, stop=True)), reads=[Bon, Bw], writes=[BPS[pb]])
        for it in range(iters):
            pb = it % 2
            S.op(S.dve, ("tensor_tensor", dict(out=mid, in0=lo, in1=hi, op=ALU.add)), reads=[Bw], writes=[Bw])
            S.op(S.dve, ("tensor_scalar", dict(out=mid, in0=mid, scalar1=0.5, scalar2=None, op0=ALU.mult)), reads=[Bw], writes=[Bw])
            count(mid, pb)
            S.op(S.dve, ("tensor_scalar", dict(out=ge, in0=PS[pb][:, 0:NBE], scalar1=float(KCAP) - 0.5, scalar2=None, op0=ALU.is_ge)), reads=[BPS[pb]], writes=[Bw])
            S.op(S.dve, ("tensor_tensor", dict(out=t1, in0=ge, in1=mid, op=ALU.mult)), reads=[Bw], writes=[Bw])
            S.op(S.dve, ("tensor_tensor", dict(out=lo, in0=lo, in1=t1, op=ALU.max)), reads=[Bw], writes=[Bw])
            S.op(S.dve, ("scalar_tensor_tensor", dict(out=t2, in0=ge, scalar=4.0, in1=mid, op0=ALU.mult, op1=ALU.add)), reads=[Bw], writes=[Bw])
            S.op(S.dve, ("tensor_tensor", dict(out=hi, in0=hi, in1=t2, op=ALU.min)), reads=[Bw], writes=[Bw])
        count(lo, 0)
        S.op(S.dve, ("tensor_copy", dict(out=t1, in_=PS[0][:, 0:NBE])), reads=[BPS[0]], writes=[Bw])
        Bo = Buf()
        S.dma(S.sp, m_out[:, :], cmp_[:], reads=[Bcmp], writes=[Bo])
        S.dma(S.sp, c_out[:, :], t1, reads=[Bw], writes=[Bo])
        S.finish([Bo])
        S.emit()
    return nc


def build_experts(NJ, CAP, FF=2048):
    nc = bass.Bass("TRN2", target_bir_lowering=False)
    NW = 2
    JPW = NJ // NW
    NTT = CAP // 128
    NTB = CAP // 512
    FC = FF // 128
    XT = nc.dram_tensor("XT", [NJ, D, CAP], BF16, kind="ExternalInput").ap()
    gates = nc.dram_tensor("gates", [128, NJ * NTT], F32, kind="ExternalInput").ap()
    wg = nc.dram_tensor("wg", [NW, D, FF], F32, kind="ExternalInput").ap()
    wu = nc.dram_tensor("wu", [NW, D, FF], F32, kind="ExternalInput").ap()
    wd = nc.dram_tensor("wd", [NW, FF, D], F32, kind="ExternalInput").ap()
    Y = nc.dram_tensor("Y", [NJ, CAP, D], BF16, kind="ExternalOutput").ap()
    with contextlib.ExitStack() as st:
        S = Sched(nc, st)
        PS = [S.ps(f"ps{i}", [128, 512]) for i in range(8)]
        BPS = [Buf(f"ps{i}", excl=True) for i in range(8)]
        gt = S.sb("gt", [128, NJ * NTT]); Bgt = Buf()
        S.dma(S.sp, gt[:], gates[:, :], writes=[Bgt])
        xw = S.sb("xw", [128, KC * max(CAP, D)], BF16); Bxw = Buf()
        hid = S.sb("hid", [128, FC * CAP], BF16); Bhid = Buf()
        wgs = [S.sb(f"wgs{i}", [128, KC * 128], BF16) for i in range(2)]; Bwgs = [Buf(), Buf()]
        wus = [S.sb(f"wus{i}", [128, KC * 128], BF16) for i in range(2)]; Bwus = [Buf(), Buf()]
        stgf = [S.sb(f"stgf{i}", [128, KC * 128], F32) for i in range(4)]; Bstgf = [Buf() for _ in range(4)]
        nsf = [0]
        def load_cast(dst_ap, Bdst, src_ap, view=None):
            k = nsf[0] % 4; nsf[0] += 1
            sv = stgf[k][:] if view is None else view(stgf[k])
            S.dma(S.sp if k % 2 == 0 else S.act, sv, src_ap, writes=[Bstgf[k]])
            S.op(S.pool, ("tensor_copy", dict(out=dst_ap, in_=sv)), reads=[Bstgf[k]], writes=[Bdst])
        tmp = [S.sb(f"tmp{i}", [128, 512]) for i in range(2)]; Btmp = [Buf(), Buf()]
        yb = [S.sb(f"yb{i}", [128, D], BF16) for i in range(2)]; Byb = [Buf(), Buf()]
        Bout = Buf()
        xv = xw[:, 0:KC * CAP].rearrange("p (kc t) -> p kc t", t=CAP)
        wdv = xw[:, 0:FC * D].rearrange("p (fc n) -> p fc n", n=D)
        hv = hid[:].rearrange("p (fc t) -> p fc t", t=CAP)
        nt = 0; ny = 0
        for j in range(NJ):
            e = j // JPW
            S.dma(S.sp, xv, XT[j].rearrange("(kc p) t -> p kc t", p=128), writes=[Bxw])
            for fc in range(FC):
                wb = fc % 2
                v3_ = lambda tl: tl[:].rearrange("p (kc f) -> p kc f", f=128)
                load_cast(v3_(wgs[wb]), Bwgs[wb], wg[e, :, fc * 128:(fc + 1) * 128].rearrange("(kc p) f -> p kc f", p=128), view=v3_)
                load_cast(v3_(wus[wb]), Bwus[wb], wu[e, :, fc * 128:(fc + 1) * 128].rearrange("(kc p) f -> p kc f", p=128), view=v3_)
                for tb in range(NTB):
                    for kc in range(KC):
                        S.op(S.pe, ("matmul", dict(out=PS[tb % 4][:, :], lhsT=wgs[wb][:, kc * 128:(kc + 1) * 128], rhs=xv[:, kc, tb * 512:(tb + 1) * 512],
                                                   start=(kc == 0), stop=(kc == KC - 1))), reads=[Bwgs[wb], Bxw], writes=[BPS[tb % 4]])
                    for kc in range(KC):
                        S.op(S.pe, ("matmul", dict(out=PS[4 + tb % 4][:, :], lhsT=wus[wb][:, kc * 128:(kc + 1) * 128], rhs=xv[:, kc, tb * 512:(tb + 1) * 512],
                                                   start=(kc == 0), stop=(kc == KC - 1))), reads=[Bwus[wb], Bxw], writes=[BPS[4 + tb % 4]])
                    ti = nt % 2; nt += 1
                    S.op(S.act, ("activation", dict(out=tmp[ti][:], in_=PS[tb % 4][:, :], func=AF.Silu)), reads=[BPS[tb % 4]], writes=[Btmp[ti]])
                    S.op(S.dve, ("tensor_tensor", dict(out=hv[:, fc, tb * 512:(tb + 1) * 512], in0=tmp[ti][:], in1=PS[4 + tb % 4][:, :], op=ALU.mult)),
                         reads=[Btmp[ti], BPS[4 + tb % 4]], writes=[Bhid])
            for fc in range(FC):
                load_cast(wdv[:, fc, :], Bxw, wd[e, fc * 128:(fc + 1) * 128, :])
            for tt in range(NTT):
                for nb in range(4):
                    for fc in range(FC):
                        S.op(S.pe, ("matmul", dict(out=PS[nb][:, :], lhsT=hv[:, fc, tt * 128:(tt + 1) * 128], rhs=wdv[:, fc, nb * 512:(nb + 1) * 512],
                                                   start=(fc == 0), stop=(fc == FC - 1))), reads=[Bhid, Bxw], writes=[BPS[nb]])
                yi = ny % 2; ny += 1
                gcol = gt[:, j * NTT + tt:j * NTT + tt + 1]
                for nb in range(4):
                    sl = slice(nb * 512, (nb + 1) * 512)
                    if nb % 2 == 0:
                        S.op(S.act, ("activation", dict(out=yb[yi][:, sl], in_=PS[nb][:, :], func=AF.Copy, scale=gcol)), reads=[BPS[nb], Bgt], writes=[Byb[yi]])
                    else:
                        S.op(S.dve, ("tensor_scalar", dict(out=yb[yi][:, sl], in0=PS[nb][:, :], scalar1=gcol, scalar2=None, op0=ALU.mult)), reads=[BPS[nb], Bgt], writes=[Byb[yi]])
                S.dma(S.sp, Y[j, tt * 128:(tt + 1) * 128, :], yb[yi][:], reads=[Byb[yi]], writes=[Bout])
        S.finish([Bout])
        S.emit()
    return nc


import ml_dtypes as _mld
_BF = _mld.bfloat16
_B, _T, _DEPTH = 2, 16384, 2
_TC = 4096
_NEXP, _CAP = 16, 2048
_prog_cache = {}


def _make_consts():
    c = np.zeros((128, 768), np.float32)
    c[:, 0:128] = np.eye(128)
    c[:, 128:256] = 1.0
    s = np.arange(64)[:, None]; t = np.arange(64)[None, :]
    for d in range(2):
        ok = (s <= t) if d == 0 else (s >= t)
        strict = (s < t) if d == 0 else (s > t)
        c[0:64, 256 + d * 64: 320 + d * 64] = ok
        c[0:64, 384 + d * 64: 448 + d * 64] = -1.0 * ok
        c[0:64, 512 + d * 64: 576 + d * 64] = np.where(ok, 0.0, -30000.0)
        c[0:64, 640 + d * 64: 704 + d * 64] = strict
    return c


def _prog(key, fn):
    if key not in _prog_cache:
        _prog_cache[key] = fn()
    return _prog_cache[key]


import time as _time
import sys as _sys
_T0 = [None]


def _log(msg):
    if _T0[0] is None:
        _T0[0] = _time.time()
    print(f"[kernel +{_time.time() - _T0[0]:7.1f}s] {msg}", file=_sys.stderr, flush=True)


def _run(nc, in_maps, tag=""):
    nb = sum(int(np.asarray(v).nbytes) for m in in_maps for v in m.values())
    t0 = _time.time()
    res = run_bass_kernel_spmd(nc, in_maps, core_ids=list(range(len(in_maps))))
    _log(f"launch {tag}: {nb / 1e6:.0f} MB in, {_time.time() - t0:.1f}s")
    return res.results


def _u16(a):
    return np.asarray(a).view(np.uint16)


def _bcast(v):
    return np.ascontiguousarray(np.broadcast_to(np.asarray(v, np.float32)[None, :], (128, v.shape[0])))


def kernel(x, norm_mix, norm_ffn, norm_final, w_in, conv_w, gdn_a_log, gdn_dt_bias, gdn_norm,
           hgrn_lower_bounds, hgrn_norm, w_out, w_router, w_gate, w_up, w_down):
    f32 = np.float32
    x = np.asarray(x, f32)
    norm_mix = np.asarray(norm_mix, f32); norm_ffn = np.asarray(norm_ffn, f32); norm_final = np.asarray(norm_final, f32)
    w_in = np.asarray(w_in, f32); conv_w = np.asarray(conv_w, f32)
    gdn_a_log = np.asarray(gdn_a_log, f32); gdn_dt_bias = np.asarray(gdn_dt_bias, f32)
    gdn_norm = np.asarray(gdn_norm, f32); hgrn_norm = np.asarray(hgrn_norm, f32)
    hgrn_lower_bounds = np.asarray(hgrn_lower_bounds, f32)
    w_out = np.asarray(w_out, f32); w_router = np.asarray(w_router, f32)
    w_gate = np.asarray(w_gate, f32); w_up = np.asarray(w_up, f32); w_down = np.asarray(w_down, f32)
    B, T, TC = _B, _T, _TC
    consts = _make_consts()
    ident = np.eye(128, dtype=f32)
    ones = np.ones((128, 128), f32)
    iota = np.ascontiguousarray(np.broadcast_to(np.arange(128, dtype=f32)[None, :], (128, 128)))
    tok_shard = lambda a, c: np.ascontiguousarray(a[c // 4, (c % 4) * TC:((c % 4) + 1) * TC])

    ncN = _prog(("N",), lambda: build_tok(TC, "N"))
    nwb = _bcast(norm_mix[0])
    _log("start")
    res = _run(ncN, [{"h": tok_shard(x, c), "nwb": nwb} for c in range(8)], "N")
    u = np.stack([np.concatenate([_u16(res[b * 4 + q]["u_out"]) for q in range(4)], axis=0) for b in range(B)])
    h = x
    out = None
    for l in range(_DEPTH):
        ncM, _ = _prog(("M",), lambda: build_mixer(T, 2, 2, depth=_DEPTH))
        uT = [np.ascontiguousarray(u[b].T).view(_BF) for b in range(B)]
        hcm = np.ascontiguousarray(np.broadcast_to((np.arange(_DEPTH) <= l).astype(f32)[None, :], (128, _DEPTH)))
        in_maps = []
        for c in range(8):
            b, g = c // 4, c % 4
            heads = (2 * g, 2 * g + 1)
            fm_cols = []
            for hh in heads:
                fm_cols += list(range(hh * 128, hh * 128 + 128)) + list(range(1024 + hh * 128, 1024 + hh * 128 + 128)) + list(range(2048 + hh * 128, 2048 + hh * 128 + 128))
                fm_cols += [4096 + hh, 4096 + 8 + hh, 4112 + hh, 4112 + 8 + hh]
            for hh in heads:
                fm_cols += list(range(4128 + hh * 128, 4128 + hh * 128 + 128))
                fm_cols += list(range(5152 + hh * 128, 5152 + hh * 128 + 128)) + list(range(5152 + 1024 + hh * 128, 5152 + 1024 + hh * 128 + 128))
            tm_cols = []
            for hh in heads:
                tm_cols += list(range(3072 + hh * 128, 3072 + hh * 128 + 128))
            for hh in heads:
                tm_cols += list(range(7200 + hh * 128, 7200 + hh * 128 + 128)) + list(range(8224 + hh * 128, 8224 + hh * 128 + 128))
            wfm = np.ascontiguousarray(w_in[l][:, fm_cols])
            wtm = np.ascontiguousarray(w_in[l][:, tm_cols])
            gcw = np.zeros((128, 2 * 21), f32)
            gpar = np.zeros((2, 4), f32)
            hlb = np.zeros((128, 2 * 2 * _DEPTH), f32)
            for hi, hh in enumerate(heads):
                for j in range(3):
                    gcw[:, hi * 21 + j * 7: hi * 21 + j * 7 + 7] = conv_w[l][:, j * 1024 + hh * 128: j * 1024 + hh * 128 + 128].T
                gpar[:, hi * 2 + 0] = gdn_dt_bias[l][:, hh]
                gpar[:, hi * 2 + 1] = gdn_a_log[l][:, hh]
                for d in range(2):
                    for dep in range(_DEPTH):
                        hlb[:, (hi * 2 + d) * _DEPTH + dep] = hgrn_lower_bounds[dep, d, hh * 128:(hh + 1) * 128]
            normw = np.ascontiguousarray(np.broadcast_to(np.concatenate([gdn_norm[l], hgrn_norm[l]])[None, :], (128, 256)))
            in_maps.append({"uT": uT[b], "wfm": wfm, "wtm": wtm, "consts": consts, "gcw": gcw, "gpar": gpar, "normw": normw, "hlb": hlb, "hcm": hcm})
        res = _run(ncM, in_maps, f"M{l}")
        mixed = np.zeros((B, T, 2048), np.uint16)
        for c in range(8):
            b, g = c // 4, c % 4
            om = _u16(res[c]["omix"])
            mixed[b, :, (2 * g) * 128:(2 * g + 2) * 128] = om[:, 0:256]
            mixed[b, :, 1024 + (2 * g) * 128:1024 + (2 * g + 2) * 128] = om[:, 256:512]
        del res
        ncO = _prog(("O",), lambda: build_tok(TC, "O"))
        nwb = _bcast(norm_ffn[l])
        in_maps = []
        for c in range(8):
            b, q = c // 4, c % 4
            in_maps.append({"h": tok_shard(h, c), "nwb": nwb, "oT": np.ascontiguousarray(mixed[b, q * TC:(q + 1) * TC].T).view(_BF),
                            "w_out": w_out[l], "w_r": w_router[l], "ident": ident})
        res = _run(ncO, in_maps, f"O{l}")
        u2 = np.stack([np.concatenate([_u16(res[b * 4 + q]["u_out"]) for q in range(4)], axis=0) for b in range(B)])
        probs = np.stack([np.concatenate([res[b * 4 + q]["p_out"] for q in range(4)], axis=0) for b in range(B)])
        del res
        ncK = _prog(("K",), lambda: build_topk(B * _NEXP, 128, _CAP))
        P = np.ascontiguousarray(probs.reshape(B, 128, 128, _NEXP).transpose(1, 0, 3, 2)).reshape(128, B * _NEXP * 128)
        res = _run(ncK, [{"P": P, "ones": ones}], f"K{l}")
        mask = res[0]["mask"].reshape(128, B, _NEXP, 128).transpose(1, 0, 3, 2).reshape(B, T, _NEXP) > 0.5
        idx = np.zeros((B, _NEXP, _CAP), np.int64)
        for b in range(B):
            for e in range(_NEXP):
                sel = np.nonzero(mask[b, :, e])[0]
                if sel.shape[0] != _CAP:
                    pv = probs[b, :, e]
                    order = np.lexsort((np.arange(T), -pv))
                    sel = np.sort(order[:_CAP])
                idx[b, e] = sel
        ncE = _prog(("E",), lambda: build_experts(4, _CAP))
        in_maps = []
        for c in range(8):
            XT = np.zeros((4, 2048, _CAP), np.uint16)
            gates = np.zeros((128, 4 * (_CAP // 128)), f32)
            for j in range(4):
                e = 2 * c + j // 2; b = j % 2
                XT[j] = u2[b][idx[b, e]].T
                gv = probs[b, idx[b, e], e]
                gates[:, j * (_CAP // 128):(j + 1) * (_CAP // 128)] = gv.reshape(_CAP // 128, 128).T
            in_maps.append({"XT": XT.view(_BF), "gates": gates, "wg": np.ascontiguousarray(w_gate[l, 2 * c:2 * c + 2]),
                            "wu": np.ascontiguousarray(w_up[l, 2 * c:2 * c + 2]), "wd": np.ascontiguousarray(w_down[l, 2 * c:2 * c + 2])})
        _log("E inputs ready")
        res = _run(ncE, in_maps, f"E{l}")
        del in_maps
        NTB_ = T // 128
        packs = []
        kch = 1
        for b in range(B):
            Ycat = np.concatenate([_u16(res[e // 2]["Y"])[(e % 2) * 2 + b] for e in range(_NEXP)], axis=0)
            tok = idx[b].reshape(-1)
            order = np.argsort(tok, kind="stable")
            tok_s = tok[order]
            tile = tok_s // 128
            start = np.searchsorted(tile, np.arange(NTB_), side="left")
            rank = np.arange(tok_s.shape[0]) - start[tile]
            kch = max(kch, int((rank.max() // 128) + 1))
            packs.append((Ycat, order, tok_s, tile, rank))
        del res
        last = (l == _DEPTH - 1)
        ncC = _prog(("C", kch, last), lambda: build_tok(TC, "C", KCH=kch, last=last))
        nwb = _bcast(norm_final if last else norm_mix[l + 1])
        in_maps = []
        for b in range(B):
            Ycat, order, tok_s, tile, rank = packs[b]
            rows = np.zeros((NTB_, kch, 128, 2048), np.uint16)
            rows[tile, rank // 128, rank % 128] = Ycat[order]
            ixa = np.full((NTB_, kch, 128), -1.0, f32)
            ixa[tile, rank // 128, rank % 128] = (tok_s % 128).astype(f32)
            for q in range(4):
                tl = slice(q * 32, (q + 1) * 32)
                in_maps.append({"h": np.ascontiguousarray(h[b, q * TC:(q + 1) * TC]), "nwb": nwb,
                                "oT": np.ascontiguousarray(mixed[b, q * TC:(q + 1) * TC].T).view(_BF), "w_out": w_out[l],
                                "rows": np.ascontiguousarray(rows[tl]).view(_BF),
                                "idx": np.ascontiguousarray(ixa[tl].transpose(2, 0, 1)).reshape(128, 32 * kch),
                                "iota": iota})
            del rows
        del packs
        _log(f"C inputs ready kch={kch}")
        res = _run(ncC, in_maps, f"C{l}")
        del in_maps
        if last:
            out = np.stack([np.concatenate([res[b * 4 + q]["f_out"] for q in range(4)], axis=0) for b in range(B)]).astype(f32)
        else:
            h = np.stack([np.concatenate([res[b * 4 + q]["h_out"] for q in range(4)], axis=0) for b in range(B)])
            u = np.stack([np.concatenate([_u16(res[b * 4 + q]["u_out"]) for q in range(4)], axis=0) for b in range(B)])
        del res
    return out
```
